# Optimizing a Trainium2 kernel written in Bass

```python
import numpy as np
import jax
import jax.numpy as jnp
from jax import lax

D_MODEL = 1024
BATCH = 4
SEQ = 8192
DEPTH = 4

f32 = jnp.float32

CHUNK = 64
Q_BLOCK = 128
ROPE_THETA = 10000.0
LN_EPS = 1e-5
RMS_EPS = 1e-6
NEG_INF = -1e30

ATTN_HEAD_DIM = 64

GLA_HEADS = 4
GLA_DK = 64
GLA_DV = 128
GLA_GATE_RANK = 16
GLA_TAU = 16.0

DIFF_HEADS = 4
DIFF_DH = ATTN_HEAD_DIM
DIFF_DV = 2 * DIFF_DH

SWA_Q_HEADS = 16
SWA_KV_HEADS = 2
SWA_DH = ATTN_HEAD_DIM
SWA_GROUP = SWA_Q_HEADS // SWA_KV_HEADS
WINDOW = 128
WINDOW_CHUNKS = WINDOW // CHUNK

MEM_LEN = 256
MEM_HEADS = 4
MEM_DH = D_MODEL // MEM_HEADS

D_FF = 2816
CONV_WIDTH = 3

DEEPNORM_ALPHA = (2 * DEPTH) ** 0.25
DEEPNORM_BETA = (8 * DEPTH) ** -0.25

N_EVEN = (DEPTH + 1) // 2
N_ODD = DEPTH // 2

EVEN_SPLITS = [GLA_HEADS * GLA_DK, GLA_HEADS * GLA_DK, GLA_HEADS * GLA_DV, GLA_HEADS * GLA_DV,
               GLA_GATE_RANK, DIFF_HEADS * 2 * DIFF_DH, DIFF_HEADS * 2 * DIFF_DH, DIFF_HEADS * DIFF_DV]
EVEN_IN = sum(EVEN_SPLITS)
EVEN_MIX = GLA_HEADS * GLA_DV + DIFF_HEADS * DIFF_DV
ODD_SPLITS = [SWA_Q_HEADS * SWA_DH, SWA_KV_HEADS * SWA_DH, SWA_KV_HEADS * SWA_DH]
ODD_IN = sum(ODD_SPLITS)
ODD_MIX = SWA_Q_HEADS * SWA_DH

kernel_name = "hybrid_gla_diff_swa_streaming_encoder"


def _split(t, sizes):
    return jnp.split(t, np.cumsum(sizes)[:-1].tolist(), axis=-1)


def layer_norm(x, g, b):
    xf = x.astype(f32)
    mu = jnp.mean(xf, -1, keepdims=True)
    var = jnp.mean(jnp.square(xf - mu), -1, keepdims=True)
    return ((xf - mu) * lax.rsqrt(var + LN_EPS) * g.astype(f32) + b.astype(f32)).astype(x.dtype)


def rms_norm(x, g):
    xf = x.astype(f32)
    return xf * lax.rsqrt(jnp.mean(xf * xf, -1, keepdims=True) + RMS_EPS) * g.astype(f32)


def rope_tables(positions, dim):
    inv_freq = ROPE_THETA ** (-jnp.arange(0, dim, 2, dtype=f32) / dim)
    ang = positions.astype(f32)[..., None] * inv_freq
    return jnp.cos(ang), jnp.sin(ang)


def apply_rope(t, cos, sin):
    t = t.astype(f32)
    t1, t2 = jnp.split(t, 2, axis=-1)
    c = cos[:, :, None, :]
    s = sin[:, :, None, :]
    return jnp.concatenate([t1 * c - t2 * s, t2 * c + t1 * s], axis=-1)


def gla_mixer(q, k, v, r, gate_code, gate_w, gate_b, norm_g):
    B, S, H, DK = q.shape
    DV = v.shape[-1]
    N = S // CHUNK
    log_a = jax.nn.log_sigmoid((gate_code @ gate_w + gate_b).astype(f32)) / GLA_TAU
    b = jnp.cumsum(log_a.reshape(B, N, CHUNK, H, DK), axis=2)
    qf = q.astype(f32).reshape(B, N, CHUNK, H, DK) * (DK ** -0.5)
    kf = k.astype(f32).reshape(B, N, CHUNK, H, DK)
    vf = v.astype(f32).reshape(B, N, CHUNK, H, DV)
    q_dec = qf * jnp.exp(b)
    causal = jnp.tril(jnp.ones((CHUNK, CHUNK), dtype=bool))
    a_intra = jnp.where(causal, jnp.einsum('bnihd,bnjhd->bnhij', q_dec, kf * jnp.exp(-b)), 0.0)
    o = jnp.einsum('bnhij,bnjhe->bnihe', a_intra, vf)
    b_last = b[:, :, -1]
    kv = jnp.einsum('bnchd,bnche->nbhde', kf * jnp.exp(b_last[:, :, None] - b), vf)
    decay = jnp.moveaxis(jnp.exp(b_last), 1, 0)

    def step(state, inp):
        kv_n, dec_n = inp
        return dec_n[..., None] * state + kv_n, state

    _, states = lax.scan(step, jnp.zeros((B, H, DK, DV), f32), (kv, decay))
    o = o + jnp.einsum('bnihd,nbhde->bnihe', q_dec, states)
    o = o.reshape(B, S, H, DV)
    return rms_norm(o, norm_g) * jax.nn.silu(r.astype(f32))


def diff_attention(q, k, v, lq1, lk1, lq2, lk2, norm_g, lam_init, cos, sin):
    B, S = q.shape[0], q.shape[1]
    q = apply_rope(q, cos, sin).reshape(B, S, DIFF_HEADS, 2, DIFF_DH)
    k = apply_rope(k, cos, sin).reshape(B, S, DIFF_HEADS, 2, DIFF_DH)
    vf = v.astype(f32)
    lam = (jnp.exp(jnp.sum(lq1.astype(f32) * lk1.astype(f32)))
           - jnp.exp(jnp.sum(lq2.astype(f32) * lk2.astype(f32))) + lam_init)
    n_blocks = S // Q_BLOCK
    q_blocks = jnp.moveaxis(q.reshape(B, n_blocks, Q_BLOCK, DIFF_HEADS, 2, DIFF_DH), 1, 0)
    key_chunk = jnp.arange(S) // CHUNK
    scale = DIFF_DH ** -0.5

    def one_block(args):
        q_blk, blk = args
        s = jnp.einsum('bqhtd,bkhtd->bhtqk', q_blk, k) * scale
        q_chunk = (blk * Q_BLOCK + jnp.arange(Q_BLOCK)) // CHUNK
        visible = key_chunk[None, :] <= q_chunk[:, None]
        p = jax.nn.softmax(jnp.where(visible, s, NEG_INF), axis=-1)
        a = p[:, :, 0] - lam * p[:, :, 1]
        return jnp.einsum('bhqk,bkhe->bqhe', a, vf)

    o = lax.map(one_block, (q_blocks, jnp.arange(n_blocks)))
    o = jnp.moveaxis(o, 0, 1).reshape(B, S, DIFF_HEADS, DIFF_DV)
    return rms_norm(o, norm_g) * (1.0 - lam_init)


def banded_chunks(t):
    B, S, H, D = t.shape
    N = S // CHUNK
    tp = jnp.pad(t, ((0, 0), (WINDOW_CHUNKS * CHUNK, 0), (0, 0), (0, 0)))
    tp = tp.reshape(B, N + WINDOW_CHUNKS, CHUNK, H, D)
    return jnp.concatenate([tp[:, j:j + N] for j in range(WINDOW_CHUNKS + 1)], axis=2)


def swa_sink_attention(q, k, v, sinks):
    B, S = q.shape[0], q.shape[1]
    N = S // CHUNK
    qc = q.reshape(B, N, CHUNK, SWA_KV_HEADS, SWA_GROUP, SWA_DH)
    kb = banded_chunks(k)
    vb = banded_chunks(v)
    s = jnp.einsum('bnikgd,bnjkd->bnkgij', qc, kb) * (SWA_DH ** -0.5)
    key_chunk = (jnp.arange(N)[:, None] - WINDOW_CHUNKS
                 + (jnp.arange((WINDOW_CHUNKS + 1) * CHUNK) // CHUNK)[None, :])
    s = jnp.where((key_chunk >= 0)[None, :, None, None, None, :], s, NEG_INF)
    sink = sinks.astype(f32).reshape(SWA_KV_HEADS, SWA_GROUP)[None, None, :, :, None, None]
    m = jnp.maximum(jnp.max(s, -1, keepdims=True), sink)
    p = jnp.exp(s - m)
    p = p / (jnp.sum(p, -1, keepdims=True) + jnp.exp(sink - m))
    o = jnp.einsum('bnkgij,bnjkd->bnikgd', p, vb)
    return o.reshape(B, S, SWA_Q_HEADS, SWA_DH)


def even_mixer(x, cos, sin, w_in, w_out, gate_w, gate_b, gla_g, lq1, lk1, lq2, lk2, diff_g, lam_init):
    B, S, _ = x.shape
    qa, ka, va, ra, ga, qb, kb, vb = _split(x @ w_in, EVEN_SPLITS)
    o_a = gla_mixer(qa.reshape(B, S, GLA_HEADS, GLA_DK), ka.reshape(B, S, GLA_HEADS, GLA_DK),
                    va.reshape(B, S, GLA_HEADS, GLA_DV), ra.reshape(B, S, GLA_HEADS, GLA_DV),
                    ga, gate_w, gate_b, gla_g)
    o_b = diff_attention(qb.reshape(B, S, DIFF_HEADS * 2, DIFF_DH), kb.reshape(B, S, DIFF_HEADS * 2, DIFF_DH),
                         vb.reshape(B, S, DIFF_HEADS, DIFF_DV), lq1, lk1, lq2, lk2, diff_g, lam_init, cos, sin)
    o = jnp.concatenate([o_a.reshape(B, S, -1), o_b.reshape(B, S, -1)], axis=-1)
    return o.astype(x.dtype) @ w_out


def odd_mixer(x, cos, sin, w_in, w_out, sinks):
    B, S, _ = x.shape
    q, k, v = _split(x @ w_in, ODD_SPLITS)
    q = apply_rope(q.reshape(B, S, SWA_Q_HEADS, SWA_DH), cos, sin)
    k = apply_rope(k.reshape(B, S, SWA_KV_HEADS, SWA_DH), cos, sin)
    v = v.reshape(B, S, SWA_KV_HEADS, SWA_DH).astype(f32)
    o = swa_sink_attention(q, k, v, sinks)
    return o.reshape(B, S, ODD_MIX).astype(x.dtype) @ w_out


def memory_cross_attention(x, mem, w_q, w_kv, w_out):
    B, S, _ = x.shape
    M = mem.shape[1]
    q = (x @ w_q).reshape(B, S, MEM_HEADS, MEM_DH).astype(f32)
    k, v = jnp.split((mem @ w_kv).astype(f32), 2, axis=-1)
    k = k.reshape(B, M, MEM_HEADS, MEM_DH)
    v = v.reshape(B, M, MEM_HEADS, MEM_DH)
    p = jax.nn.softmax(jnp.einsum('bshd,bmhd->bhsm', q, k) * (MEM_DH ** -0.5), axis=-1)
    o = jnp.einsum('bhsm,bmhd->bshd', p, v).reshape(B, S, D_MODEL)
    return o.astype(x.dtype) @ w_out


def causal_depthwise_conv(t, w, b):
    C = t.shape[-1]
    y = lax.conv_general_dilated(t, w[:, None, :], window_strides=(1,), padding=[(CONV_WIDTH - 1, 0)],
                                 dimension_numbers=('NWC', 'WIO', 'NWC'), feature_group_count=C)
    return y + b


def conv_ffn(x, w_in, conv_w, conv_b, w_out):
    g, u = jnp.split(x @ w_in, 2, axis=-1)
    g = causal_depthwise_conv(g, conv_w, conv_b)
    h = jax.nn.gelu(g.astype(f32)) * u.astype(f32)
    return h.astype(x.dtype) @ w_out


def setup_inputs(seed: int = 0) -> dict:
    key = jax.random.key(seed)
    ks = jax.random.split(key, 32)

    def nrm(k, shape, scale):
        return jax.random.normal(k, shape, f32) * scale

    x = nrm(ks[0], (BATCH, SEQ, D_MODEL), 1.0)
    mem = nrm(ks[1], (BATCH, MEM_LEN, D_MODEL), 1.0)
    offset = jax.random.randint(ks[2], (BATCH, 1), 0, 4096, dtype=jnp.int32)
    positions = offset + jnp.arange(SEQ, dtype=jnp.int32)[None, :]
    return {
        "x": x,
        "mem": mem,
        "positions": positions,
        "even_w_in": nrm(ks[3], (N_EVEN, D_MODEL, EVEN_IN), D_MODEL ** -0.5),
        "even_w_out": nrm(ks[4], (N_EVEN, EVEN_MIX, D_MODEL), EVEN_MIX ** -0.5 * DEEPNORM_BETA),
        "gla_gate_w": nrm(ks[5], (N_EVEN, GLA_GATE_RANK, GLA_HEADS * GLA_DK), GLA_GATE_RANK ** -0.5),
        "gla_gate_b": nrm(ks[6], (N_EVEN, GLA_HEADS * GLA_DK), 0.1),
        "gla_norm_g": 1.0 + nrm(ks[7], (N_EVEN, GLA_DV), 0.02),
        "diff_lam_q1": nrm(ks[8], (N_EVEN, DIFF_DH), 0.1),
        "diff_lam_k1": nrm(ks[9], (N_EVEN, DIFF_DH), 0.1),
        "diff_lam_q2": nrm(ks[10], (N_EVEN, DIFF_DH), 0.1),
        "diff_lam_k2": nrm(ks[11], (N_EVEN, DIFF_DH), 0.1),
        "diff_norm_g": 1.0 + nrm(ks[12], (N_EVEN, DIFF_DV), 0.02),
        "odd_w_in": nrm(ks[13], (N_ODD, D_MODEL, ODD_IN), D_MODEL ** -0.5),
        "odd_w_out": nrm(ks[14], (N_ODD, ODD_MIX, D_MODEL), ODD_MIX ** -0.5 * DEEPNORM_BETA),
        "swa_sinks": nrm(ks[15], (N_ODD, SWA_Q_HEADS), 0.5),
        "mem_w_q": nrm(ks[16], (DEPTH, D_MODEL, D_MODEL), D_MODEL ** -0.5),
        "mem_w_kv": nrm(ks[17], (DEPTH, D_MODEL, 2 * D_MODEL), D_MODEL ** -0.5),
        "mem_w_out": nrm(ks[18], (DEPTH, D_MODEL, D_MODEL), D_MODEL ** -0.5 * DEEPNORM_BETA),
        "ffn_w_in": nrm(ks[19], (DEPTH, D_MODEL, 2 * D_FF), D_MODEL ** -0.5),
        "ffn_conv_w": nrm(ks[20], (DEPTH, CONV_WIDTH, D_FF), CONV_WIDTH ** -0.5),
        "ffn_conv_b": nrm(ks[21], (DEPTH, D_FF), 0.02),
        "ffn_w_out": nrm(ks[22], (DEPTH, D_FF, D_MODEL), D_FF ** -0.5 * DEEPNORM_BETA),
        "ln_g": 1.0 + nrm(ks[23], (DEPTH, 3, D_MODEL), 0.02),
        "ln_b": nrm(ks[24], (DEPTH, 3, D_MODEL), 0.02),
    }


def reference(x, mem, positions, even_w_in, even_w_out, gla_gate_w, gla_gate_b, gla_norm_g,
              diff_lam_q1, diff_lam_k1, diff_lam_q2, diff_lam_k2, diff_norm_g,
              odd_w_in, odd_w_out, swa_sinks, mem_w_q, mem_w_kv, mem_w_out,
              ffn_w_in, ffn_conv_w, ffn_conv_b, ffn_w_out, ln_g, ln_b):
    cos, sin = rope_tables(positions, ATTN_HEAD_DIM)
    for i in range(DEPTH):
        j = i // 2
        if i % 2 == 0:
            lam_init = 0.8 - 0.6 * float(np.exp(-0.3 * i))
            h = even_mixer(x, cos, sin, even_w_in[j], even_w_out[j], gla_gate_w[j], gla_gate_b[j],
                           gla_norm_g[j], diff_lam_q1[j], diff_lam_k1[j], diff_lam_q2[j], diff_lam_k2[j],
                           diff_norm_g[j], lam_init)
        else:
            h = odd_mixer(x, cos, sin, odd_w_in[j], odd_w_out[j], swa_sinks[j])
        x = layer_norm(DEEPNORM_ALPHA * x + h, ln_g[i, 0], ln_b[i, 0])
        h = memory_cross_attention(x, mem, mem_w_q[i], mem_w_kv[i], mem_w_out[i])
        x = layer_norm(DEEPNORM_ALPHA * x + h, ln_g[i, 1], ln_b[i, 1])
        h = conv_ffn(x, ffn_w_in[i], ffn_conv_w[i], ffn_conv_b[i], ffn_w_out[i])
        x = layer_norm(DEEPNORM_ALPHA * x + h, ln_g[i, 2], ln_b[i, 2])
    return x
```

```python
import numpy as np
from contextlib import ExitStack
import concourse.bass as bass
import concourse.mybir as mybir
from concourse.bass_utils import run_bass_kernel_spmd

F32 = mybir.dt.float32
BF16 = mybir.dt.bfloat16
I32 = mybir.dt.int32
ALU = mybir.AluOpType
AF = mybir.ActivationFunctionType

D = 1024
KC = 8
DEPTH = 4
DFF = 2816
NJ = DFF // 128
MEM = 256
ALPHA = float((2 * DEPTH) ** 0.25)
G = 512
GB = 256

ENGS = ("pe", "act", "dve", "pool", "sp")
NS_DMA = 8


class Tl:
    __slots__ = ("ap", "key")
    _n = 0

    def __init__(self, ap, key=None):
        self.ap = ap
        if key is None:
            Tl._n += 1
            key = "t%d" % Tl._n
        self.key = key

    def __getitem__(self, idx):
        return self.ap[idx]

    def sub(self, c):
        return Tl(self.ap[:, c, :], (self.key, c))


class _Rec:
    def __getattr__(self, name):
        def f(*a, **k):
            self.call = (name, a, k)
            return self
        return f


class Sched:
    def __init__(self, nc, es):
        self.nc = nc
        self.csem = {e: es.enter_context(nc.semaphore("c_" + e)) for e in ("pe", "act", "dve", "pool")}
        self.dsem = {q: [es.enter_context(nc.semaphore("d_%s%d" % (q, i))) for i in range(NS_DMA)]
                     for q in ("sp", "act", "pool")}
        self.ccount = {e: 0 for e in self.csem}
        self.dcount = {q: 0 for q in self.dsem}
        self._reset()

    def _reset(self):
        self.ops = {e: [] for e in ENGS}
        self.lastw = {}
        self.readers = {}
        self.kids = {}

    def _ov(self, k):
        if isinstance(k, tuple):
            self.kids.setdefault(k[0], set()).add(k)
            return (k, k[0])
        ch = self.kids.get(k)
        return (k,) + tuple(ch) if ch else (k,)

    def op(self, eng, fn, R=(), W=(), dma=False):
        ops = self.ops[eng]
        idx = len(ops)
        me = (eng, idx, dma)
        deps = []
        for r in R:
            for k in self._ov(r.key):
                deps.extend(self.lastw.get(k, ()))
        for w in W:
            for k in self._ov(w.key):
                for lw in self.lastw.get(k, ()):
                    if (lw[0] != eng or lw[2] or dma) and not (lw[2] and dma):
                        deps.append(lw)
                for rd in self.readers.get(k, ()):
                    if rd[0] != eng or rd[2] or dma:
                        deps.append(rd)
        pr = _Rec()
        fn(pr)
        rec = {"call": pr.call, "deps": deps, "dma": dma, "sig": False}
        if dma:
            rec["dn"] = self.dcount[eng]
            self.dcount[eng] += 1
        ops.append(rec)
        for r in R:
            self.readers.setdefault(r.key, []).append(me)
        for w in W:
            prev = self.lastw.get(w.key, [])
            if dma and prev and all(p[2] for p in prev) and not self.readers.get(w.key):
                self.lastw[w.key] = prev + [me]
            else:
                self.lastw[w.key] = [me]
            self.readers[w.key] = []
        return me

    def pe(self, fn, R=(), W=()):
        return self.op("pe", fn, R, W)

    def act(self, fn, R=(), W=()):
        return self.op("act", fn, R, W)

    def dve(self, fn, R=(), W=()):
        return self.op("dve", fn, R, W)

    def pool(self, fn, R=(), W=()):
        return self.op("pool", fn, R, W)

    def dma(self, q, out, in_, R=(), W=(), **kw):
        return self.op(q, lambda e: e.dma_start(out=out, in_=in_, **kw), R, W, dma=True)

    def flush(self):
        nc = self.nc
        ops = self.ops
        for e in ENGS:
            for rec in ops[e]:
                for (de, di, dd) in rec["deps"]:
                    if not dd:
                        ops[de][di]["sig"] = True
        for e in ("pe", "act", "dve", "pool"):
            c = self.ccount[e]
            for rec in ops[e]:
                if rec["sig"] and not rec["dma"]:
                    c += 1
                    rec["sv"] = c
            self.ccount[e] = c
        dsem, csem = self.dsem, self.csem

        def emit(ename, eng):
            waited_c, waited_d = {}, {}
            for rec in ops[ename]:
                for (de, di, dd) in rec["deps"]:
                    drec = ops[de][di]
                    if dd:
                        n = drec["dn"]
                        val = 16 * (n // NS_DMA + 1)
                        key = (de, n % NS_DMA)
                        if waited_d.get(key, 0) >= val:
                            continue
                        waited_d[key] = val
                        eng.wait_ge(dsem[de][n % NS_DMA], val)
                    else:
                        if waited_c.get(de, -1) >= di:
                            continue
                        waited_c[de] = di
                        eng.wait_ge(csem[de], drec["sv"])
                if rec["dma"]:
                    n = rec["dn"]
                    slot = n % NS_DMA
                    if n >= NS_DMA:
                        val = 16 * (n // NS_DMA)
                        if waited_d.get((ename, slot), 0) < val:
                            waited_d[(ename, slot)] = val
                            eng.wait_ge(dsem[ename][slot], val)
                    nm, a, k = rec["call"]
                    getattr(eng, nm)(*a, **k).then_inc(dsem[ename][slot], 16)
                else:
                    nm, a, k = rec["call"]
                    ins = getattr(eng, nm)(*a, **k)
                    if rec["sig"]:
                        ins.then_inc(csem[ename], 1)
            if ename in dsem:
                tot = self.dcount[ename]
                for slot in range(NS_DMA):
                    cnt = (tot - slot + NS_DMA - 1) // NS_DMA if tot > slot else 0
                    if cnt > 0 and waited_d.get((ename, slot), 0) < 16 * cnt:
                        eng.wait_ge(dsem[ename][slot], 16 * cnt)

        with nc.Block() as block:
            @block.sync
            def _(e):
                emit("sp", e)

            @block.tensor
            def _(e):
                emit("pe", e)

            @block.scalar
            def _(e):
                emit("act", e)

            @block.vector
            def _(e):
                emit("dve", e)

            @block.gpsimd
            def _(e):
                emit("pool", e)
        self._reset()

    def clear_sems(self):
        allsems = list(self.csem.values()) + [s for v in self.dsem.values() for s in v]
        with self.nc.Block() as block:
            @block.sync
            def _(e):
                for s in allsems:
                    e.sem_clear(s)


def _col_layout():
    off = {}
    n = 0

    def add(name, cnt):
        nonlocal n
        off[name] = n
        n += cnt
    add("ln_g", DEPTH * 3 * KC)
    add("ln_b", DEPTH * 3 * KC)
    add("gate_b", 2 * 4)
    add("gla_g", 2)
    add("diff_g", 2)
    add("conv_w", DEPTH * 3 * NJ)
    add("conv_b", DEPTH * NJ)
    add("invf", 1)
    add("sign", 1)
    return off, n


COL, NCOL = _col_layout()


def pack_cols(inp):
    c = np.zeros((128, NCOL), np.float32)
    lg = np.asarray(inp["ln_g"], np.float32).reshape(DEPTH * 3, KC, 128)
    lb = np.asarray(inp["ln_b"], np.float32).reshape(DEPTH * 3, KC, 128)
    c[:, COL["ln_g"]:COL["ln_g"] + DEPTH * 3 * KC] = lg.reshape(-1, 128).T
    c[:, COL["ln_b"]:COL["ln_b"] + DEPTH * 3 * KC] = lb.reshape(-1, 128).T
    gb = np.asarray(inp["gla_gate_b"], np.float32).reshape(2 * 4, 64)
    c[:64, COL["gate_b"]:COL["gate_b"] + 8] = gb.T
    c[:, COL["gla_g"]:COL["gla_g"] + 2] = np.asarray(inp["gla_norm_g"], np.float32).T
    c[:, COL["diff_g"]:COL["diff_g"] + 2] = np.asarray(inp["diff_norm_g"], np.float32).T
    cw = np.asarray(inp["ffn_conv_w"], np.float32).reshape(DEPTH * 3 * NJ, 128)
    c[:, COL["conv_w"]:COL["conv_w"] + DEPTH * 3 * NJ] = cw.T
    cb = np.asarray(inp["ffn_conv_b"], np.float32).reshape(DEPTH * NJ, 128)
    c[:, COL["conv_b"]:COL["conv_b"] + DEPTH * NJ] = cb.T
    p = np.arange(128)
    c[:, COL["invf"]] = (10000.0 ** (-(np.arange(0, 64, 2, dtype=np.float32)) / 64.0)).astype(np.float32)[p % 32]
    c[:, COL["sign"]] = np.where((p % 64) < 32, -1.0, 1.0)
    return c


def build(T, dbg=False, nlayers=DEPTH):
    NLW = nlayers if dbg else DEPTH
    NE, NO = (NLW + 1) // 2, max(1, NLW // 2)
    NG = T // G
    NGB = T // GB
    NT = T // 128
    nc = bass.Bass("TRN2", target_bir_lowering=False)

    def din(name, shape, dt=F32):
        return nc.dram_tensor(name, list(shape), dt, kind="ExternalInput").ap()

    def dscr(name, shape, dt):
        return nc.dram_tensor(name, list(shape), dt, kind=("ExternalOutput" if dbg else "Internal")).ap()

    xT_in = din("xT", [D, T])
    memT_in = din("memT", [D, MEM])
    pos_in = din("pos", [1, T], I32)
    cols_in = din("cols", [128, NCOL])
    even_w_in = din("even_w_in", [NE, D, 3088])
    even_w_out = din("even_w_out", [NE, D, D])
    gate_w_in = din("gla_gate_w", [NE, 16, 256])
    lamv = [din(n, [NE, 64]) for n in ("diff_lam_q1", "diff_lam_k1", "diff_lam_q2", "diff_lam_k2")]
    odd_w_in = din("odd_w_in", [NO, D, 1280])
    odd_w_out = din("odd_w_out", [NO, D, D])
    sinks_in = din("swa_sinks", [NO, 16])
    mem_w_q = din("mem_w_q", [NLW, D, D])
    mem_w_kv = din("mem_w_kv", [NLW, D, 2 * D])
    mem_w_out = din("mem_w_out", [NLW, D, D])
    ffn_w_in = din("ffn_w_in", [NLW, D, 2 * DFF])
    ffn_w_out = din("ffn_w_out", [NLW, DFF, D])
    out_ap = nc.dram_tensor("outT", [D, T], F32, kind="ExternalOutput").ap()
    dbg_aps = {}
    if dbg:
        for i in range(nlayers):
            for s in ("mix", "x2", "x3"):
                dbg_aps[(i, s)] = nc.dram_tensor("dbg_%d_%s" % (i, s), [D, T], F32, kind="ExternalOutput").ap()

    xres = dscr("xres", [D, T], F32)
    xres2 = dscr("xres2", [D, T], F32)
    xb_d = dscr("xb", [D, T], BF16)
    xb2_d = dscr("xb2", [D, T], BF16)
    mix_d = dscr("mix", [D, T], BF16)
    qd_d = dscr("qd", [512, T], BF16)
    kd_d = dscr("kd", [512, T], BF16)
    vd_d = dscr("vd", [T, 512], BF16)
    cos_d = dscr("cosT", [128, T], F32)
    sin_d = dscr("sinT", [128, T], F32)

    def fm(ap):
        return ap.rearrange("(c p) t -> p c t", p=128)

    es0 = ExitStack()
    S = Sched(nc, es0)
    S.clear_sems()

    class Stage:
        _n = 0

        def __init__(self):
            self.es = ExitStack()

        def sb(self, shape, dt, key=None):
            Stage._n += 1
            return Tl(self.es.enter_context(nc.sbuf_tensor("sb%d" % Stage._n, list(shape), dt)), key)

        def ps(self, shape=(128, 512), dt=F32):
            Stage._n += 1
            return Tl(self.es.enter_context(nc.psum_tensor("ps%d" % Stage._n, list(shape), dt)))

        def close(self):
            S.flush()
            self.es.close()

    def load_w(wt, c_dst, w_ap, K, c0, n):
        for kc in range(K // 128):
            o = 0
            while o < n:
                m = min(2048, n - o)
                S.dma("pool", wt[:, kc, c_dst + o:c_dst + o + m],
                      w_ap[kc * 128:(kc + 1) * 128, c0 + o:c0 + o + m], W=[wt])
                o += m

    def load_w_swapped(wt, c_dst, w_ap, K, c0, nhm):
        for kc in range(K // 128):
            src = w_ap[kc * 128:(kc + 1) * 128, c0:c0 + nhm * 64].rearrange("p (h t f) -> p h t f", t=2, f=32)
            dst = wt[:, kc, c_dst:c_dst + nhm * 64].rearrange("p (h t f) -> p h t f", t=2, f=32)
            S.dma("pool", dst[:, :, 0, :], src[:, :, 1, :], W=[wt])
            S.dma("pool", dst[:, :, 1, :], src[:, :, 0, :], W=[wt])

    def gemm_fm(ps_t, wt, c0, m, xt, ncols, xs=slice(None), kcs=KC):
        for kc in range(kcs):
            S.pe(lambda e, kc=kc: e.matmul(ps_t[0:m, 0:ncols], lhsT=wt[:, kc, c0:c0 + m], rhs=xt[:, kc, xs],
                                           start=(kc == 0), stop=(kc == kcs - 1)), R=[wt, xt], W=[ps_t])

    cst = Stage()
    cols = cst.sb([128, NCOL], F32)
    ones_b = cst.sb([128, 128], BF16)
    onesD = cst.sb([128, 128], F32)
    ones128 = cst.sb([128, 128], F32)
    ident_b = cst.sb([128, 128], BF16)
    ident_f = cst.sb([128, 128], F32)
    gmask = cst.sb([128, 128], F32)
    eps_ln = cst.sb([128, 1], F32)
    eps_rms = cst.sb([128, 1], F32)
    ones_col = cst.sb([128, 64], F32)

    def colap(name, idx=0, rows=128):
        c = COL[name] + idx
        return cols[0:rows, c:c + 1]

    S.dma("sp", cols[:], cols_in, W=[cols])
    S.dve(lambda e: e.memset(ones_b[:], 1.0), W=[ones_b])
    S.dve(lambda e: e.memset(onesD[:], 1.0 / D), W=[onesD])
    S.dve(lambda e: e.memset(ones128[:], 1.0 / 128), W=[ones128])
    S.dve(lambda e: e.memset(eps_ln[:], 1e-5), W=[eps_ln])
    S.dve(lambda e: e.memset(eps_rms[:], 1e-6), W=[eps_rms])
    S.dve(lambda e: e.memset(ones_col[:], 1.0), W=[ones_col])
    S.dve(lambda e: e.memset(ident_f[:], 0.0), W=[ident_f])
    S.pool(lambda e: e.affine_select(out=ident_f[:], in_=ident_f[:], pattern=[[-1, 128]], compare_op=ALU.not_equal,
                                     fill=1.0, base=0, channel_multiplier=1), R=[ident_f], W=[ident_f])
    S.dve(lambda e: e.tensor_copy(out=ident_b[:], in_=ident_f[:]), R=[ident_f], W=[ident_b])
    S.dve(lambda e: e.memset(gmask[:], 1.0), W=[gmask])
    S.pool(lambda e: e.affine_select(out=gmask[:], in_=gmask[:], pattern=[[1, 128]], compare_op=ALU.is_ge,
                                     fill=0.0, base=0, channel_multiplier=-1), R=[gmask], W=[gmask])
    S.pool(lambda e: e.affine_select(out=gmask[:, 64:128], in_=gmask[:, 64:128], pattern=[[0, 64]], compare_op=ALU.is_ge,
                                     fill=0.0, base=-64, channel_multiplier=1), R=[gmask], W=[gmask])

    st = Stage()
    TWO_PI = float(2 * np.pi)
    C1 = 6.28125
    C2 = float(2 * np.pi - 6.28125)
    s0t = []
    for _ in range(2):
        d_ = {"pi": st.sb([128, G], I32), "ang": st.sb([128, G], F32), "xt": st.sb([128, KC, G], F32)}
        for which in ("cos", "sin"):
            d_["kf" + which] = st.sb([128, G], F32)
            d_["ki" + which] = st.sb([128, G], I32)
            d_["r" + which] = st.sb([128, G], F32)
        s0t.append(d_)
    for g in range(NG):
        gs = slice(g * G, (g + 1) * G)
        pi_t, ang = s0t[g % 2]["pi"], s0t[g % 2]["ang"]
        S.dma("sp", pi_t[:], pos_in[:, gs].partition_broadcast(128), W=[pi_t])
        S.dve(lambda e, a=ang, p=pi_t: e.tensor_copy(out=a[:], in_=p[:]), R=[pi_t], W=[ang])
        S.dve(lambda e, a=ang: e.tensor_scalar(out=a[:], in0=a[:], scalar1=colap("invf"), scalar2=None, op0=ALU.mult),
              R=[ang, cols], W=[ang])
        for which, dst in (("cos", cos_d), ("sin", sin_d)):
            sh = 0.25 if which == "cos" else 0.0
            kf, ki, r = s0t[g % 2]["kf" + which], s0t[g % 2]["ki" + which], s0t[g % 2]["r" + which]
            S.dve(lambda e, kf=kf, a=ang, sh=sh: e.tensor_scalar(out=kf[:], in0=a[:], scalar1=1.0 / TWO_PI, scalar2=sh,
                                                              op0=ALU.mult, op1=ALU.add), R=[ang], W=[kf])
            S.dve(lambda e, kf=kf, ki=ki: e.tensor_copy(out=ki[:], in_=kf[:]), R=[kf], W=[ki])
            S.dve(lambda e, kf=kf, ki=ki: e.tensor_copy(out=kf[:], in_=ki[:]), R=[ki], W=[kf])
            S.dve(lambda e, kf=kf, a=ang, r=r: e.scalar_tensor_tensor(out=r[:], in0=kf[:], scalar=-C1, in1=a[:],
                                                                    op0=ALU.mult, op1=ALU.add), R=[kf, ang], W=[r])
            S.dve(lambda e, kf=kf, r=r: e.scalar_tensor_tensor(out=r[:], in0=kf[:], scalar=-C2, in1=r[:],
                                                             op0=ALU.mult, op1=ALU.add), R=[kf, r], W=[r])
            if which == "cos":
                S.dve(lambda e, r=r: e.tensor_scalar(out=r[:], in0=r[:], scalar1=float(np.pi / 2), scalar2=3.1415925,
                                                   op0=ALU.add, op1=ALU.min), R=[r], W=[r])
            else:
                S.dve(lambda e, r=r: e.tensor_scalar(out=r[:], in0=r[:], scalar1=3.1415925, scalar2=None,
                                                   op0=ALU.min), R=[r], W=[r])
            S.dve(lambda e, r=r: e.tensor_scalar(out=r[:], in0=r[:], scalar1=-3.1415925, scalar2=None, op0=ALU.max),
                  R=[r], W=[r])
            S.act(lambda e, r=r: e.activation(out=r[:], in_=r[:], func=AF.Sin), R=[r], W=[r])
            if which == "sin":
                S.dve(lambda e, r=r: e.tensor_scalar(out=r[:], in0=r[:], scalar1=colap("sign"), scalar2=None,
                                                   op0=ALU.mult), R=[r, cols], W=[r])
            S.dma("sp", dst[:, gs], r[:], R=[r])
        xt = s0t[g % 2]["xt"]
        S.dma("sp", xt[:], fm(xT_in)[:, :, gs], W=[xt])
        S.dma("sp", fm(xres)[:, :, gs], xt[:], R=[xt])
        S.dma("pool", fm(xb_d)[:, :, gs], xt[:], R=[xt])
    st.close()

    def layer_norm(st, v, i, j, ps_mean, ps_msq, out_f, out_b, tmp, N):
        sq0, mean_sb, rstd, nmr, sq1 = tmp
        for c in range(KC):
            sq = (sq0, sq1)[c % 2]
            vc = v.sub(c)
            S.act(lambda e: e.activation(out=sq[:, 0:N], in_=vc[:], func=AF.Square), R=[vc], W=[sq])
            S.pe(lambda e: e.matmul(ps_mean[:, 0:N], lhsT=onesD[:], rhs=vc[:], start=(c == 0), stop=(c == KC - 1)),
                 R=[onesD, vc], W=[ps_mean])
            S.pe(lambda e: e.matmul(ps_msq[:, 0:N], lhsT=onesD[:], rhs=sq[:, 0:N], start=(c == 0), stop=(c == KC - 1)),
                 R=[onesD, sq], W=[ps_msq])
        S.act(lambda e: e.activation(out=mean_sb[:, 0:N], in_=ps_mean[:, 0:N], func=AF.Identity), R=[ps_mean], W=[mean_sb])
        S.dve(lambda e: e.tensor_tensor(out=rstd[:, 0:N], in0=mean_sb[:, 0:N], in1=mean_sb[:, 0:N], op=ALU.mult),
              R=[mean_sb], W=[rstd])
        S.dve(lambda e: e.tensor_tensor(out=rstd[:, 0:N], in0=ps_msq[:, 0:N], in1=rstd[:, 0:N], op=ALU.subtract),
              R=[ps_msq, rstd], W=[rstd])
        S.act(lambda e: e.activation(out=rstd[:, 0:N], in_=rstd[:, 0:N], func=AF.Ln, bias=eps_ln[:]), R=[rstd, eps_ln], W=[rstd])
        S.act(lambda e: e.activation(out=rstd[:, 0:N], in_=rstd[:, 0:N], func=AF.Exp, scale=-0.5), R=[rstd], W=[rstd])
        S.dve(lambda e: e.scalar_tensor_tensor(out=nmr[:, 0:N], in0=mean_sb[:, 0:N], scalar=-1.0, in1=rstd[:, 0:N],
                                               op0=ALU.mult, op1=ALU.mult), R=[mean_sb, rstd], W=[nmr])
        gi = (i * 3 + j) * KC
        for c in range(KC):
            vc, oc = v.sub(c), out_f.sub(c)
            S.dve(lambda e: e.tensor_tensor(out=oc[:], in0=vc[:], in1=rstd[:, 0:N], op=ALU.mult), R=[vc, rstd], W=[oc])
        for c in range(KC):
            oc, ob = out_f.sub(c), out_b.sub(c)
            S.dve(lambda e: e.tensor_tensor(out=oc[:], in0=oc[:], in1=nmr[:, 0:N], op=ALU.add), R=[oc, nmr], W=[oc])
            S.act(lambda e: e.activation(out=ob[:], in_=oc[:], func=AF.Identity,
                                         scale=colap("ln_g", gi + c), bias=colap("ln_b", gi + c)), R=[oc, cols], W=[ob])
            S.pool(lambda e: e.tensor_scalar(out=oc[:], in0=oc[:], scalar1=colap("ln_g", gi + c), scalar2=colap("ln_b", gi + c),
                                             op0=ALU.mult, op1=ALU.add), R=[oc, cols], W=[oc])

    def rstd_from_msq(dst, src_ps, eps_t, rows, N):
        S.act(lambda e: e.activation(out=dst[0:rows, 0:N], in_=src_ps[0:rows, 0:N], func=AF.Ln, bias=eps_t[0:rows, :]),
              R=[src_ps, eps_t], W=[dst])
        S.act(lambda e: e.activation(out=dst[0:rows, 0:N], in_=dst[0:rows, 0:N], func=AF.Exp, scale=-0.5), R=[dst], W=[dst])

    def stage_c_even(i):
        j = i // 2
        st = Stage()
        w = even_w_in[j]
        cQA, cKA, cRA, cGA, cVA, cVB, cQB, cQBs, cKB, cKBs = 0, 256, 512, 1024, 1040, 1552, 2064, 2576, 3088, 3600
        NW = 4112
        wt = st.sb([128, KC, NW], BF16)
        load_w(wt, cQA, w, D, 0, 256)
        load_w(wt, cKA, w, D, 256, 256)
        load_w(wt, cVA, w, D, 512, 512)
        load_w(wt, cRA, w, D, 1024, 512)
        load_w(wt, cGA, w, D, 1536, 16)
        load_w(wt, cQB, w, D, 1552, 512)
        load_w(wt, cKB, w, D, 2064, 512)
        load_w(wt, cVB, w, D, 2576, 512)
        load_w_swapped(wt, cQBs, w, D, 1552, 8)
        load_w_swapped(wt, cKBs, w, D, 2064, 8)
        gw = st.sb([16, 1, 256], BF16)
        S.dma("pool", gw[:, 0, :], gate_w_in[j], W=[gw])
        ngb = st.sb([64, 4], F32)
        S.dve(lambda e: e.tensor_scalar(out=ngb[:], in0=cols[0:64, COL["gate_b"] + 4 * j:COL["gate_b"] + 4 * j + 4],
                                        scalar1=-1.0, scalar2=None, op0=ALU.mult), R=[cols], W=[ngb])
        banks = [st.ps() for _ in range(6)]
        pmisc = st.ps()
        ptr = st.ps([128, 4, 128], BF16)
        Sf = [st.sb([64, 128], F32) for _ in range(4)]
        Sb = [st.sb([64, 128], BF16) for _ in range(4)]
        for h in range(4):
            S.dve(lambda e, h=h: e.memset(Sf[h][:], 0.0), W=[Sf[h]])
            S.dve(lambda e, h=h: e.memset(Sb[h][:], 0.0), W=[Sb[h]])
        xg = [st.sb([128, KC, G], BF16) for _ in range(2)]
        csg = [(st.sb([128, G], F32), st.sb([128, G], F32)) for _ in range(2)]
        t1 = [st.sb([128, G], F32) for _ in range(2)]
        t2 = [st.sb([128, G], F32) for _ in range(2)]
        qkb = [st.sb([128, G], BF16) for _ in range(2)]
        vtok = [st.sb([128, 512], BF16) for _ in range(2)]
        va = st.sb([128, 4, 512], BF16)
        silur = st.sb([128, 4, G], F32)
        gab = st.sb([16, 1, G], BF16)
        ex = st.sb([64, G], F32)
        cum = st.sb([64, G], F32)
        eb = [st.sb([64, G], F32) for _ in range(4)]
        enb = st.sb([64, G], F32)
        kdf = st.sb([64, G], F32)
        qdec = [st.sb([64, G], BF16) for _ in range(4)]
        kdec = [st.sb([64, G], BF16) for _ in range(4)]
        kend = st.sb([64, G], BF16)
        kendT = st.sb([128, 4, 4, 64], BF16)
        atm = [st.sb([128, 128], BF16) for _ in range(2)]
        osq = st.sb([128, G], F32)
        orstd = st.sb([128, G], F32)
        ot = st.sb([128, G], F32)
        omix = [st.sb([128, G], BF16) for _ in range(2)]
        bk = [0]

        def nb():
            b = banks[bk[0] % 4]
            bk[0] += 1
            return b
        po = banks[4]
        pq = banks[5]

        def load_g(g):
            gs = slice(g * G, (g + 1) * G)
            S.dma("sp", xg[g % 2][:], fm(xb_d)[:, :, gs], W=[xg[g % 2]])
            S.dma("sp", csg[g % 2][0][:], cos_d[:, gs], W=[csg[g % 2][0]])
            S.dma("sp", csg[g % 2][1][:], sin_d[:, gs], W=[csg[g % 2][1]])

        load_g(0)
        for g in range(NG):
            gs = slice(g * G, (g + 1) * G)
            if g + 1 < NG:
                load_g(g + 1)
            x = xg[g % 2]
            cs, sn = csg[g % 2]
            for (c_n, c_s, dst) in ((cQB, cQBs, qd_d), (cKB, cKBs, kd_d)):
                for m in range(4):
                    p1, p2 = nb(), nb()
                    gemm_fm(p1, wt, c_n + m * 128, 128, x, G)
                    gemm_fm(p2, wt, c_s + m * 128, 128, x, G)
                    a, b_, o = t1[m % 2], t2[m % 2], qkb[m % 2]
                    S.dve(lambda e, a=a, p1=p1: e.tensor_tensor(out=a[:], in0=p1[:], in1=cs[:], op=ALU.mult), R=[p1, cs], W=[a])
                    S.dve(lambda e, b_=b_, p2=p2: e.tensor_tensor(out=b_[:], in0=p2[:], in1=sn[:], op=ALU.mult), R=[p2, sn], W=[b_])
                    S.pool(lambda e, a=a, b_=b_, o=o: e.tensor_tensor(out=o[:], in0=a[:], in1=b_[:], op=ALU.add), R=[a, b_], W=[o])
                    S.dma("sp", dst[m * 128:(m + 1) * 128, gs], o[:], R=[o])
            for tt in range(4):
                ts_ = slice(tt * 128, (tt + 1) * 128)
                pv = nb()
                for kc in range(KC):
                    S.pe(lambda e, kc=kc, pv=pv, ts_=ts_: e.matmul(pv[:, :], lhsT=x[:, kc, ts_], rhs=wt[:, kc, cVB:cVB + 512],
                                                                   start=(kc == 0), stop=(kc == KC - 1)), R=[x, wt], W=[pv])
                vt = vtok[tt % 2]
                S.act(lambda e, vt=vt, pv=pv: e.activation(out=vt[:], in_=pv[:], func=AF.Identity), R=[pv], W=[vt])
                S.dma("sp", vd_d[g * G + tt * 128:g * G + (tt + 1) * 128, :], vt[:], R=[vt])
                pv2 = nb()
                for kc in range(KC):
                    S.pe(lambda e, kc=kc, pv2=pv2, ts_=ts_: e.matmul(pv2[:, :], lhsT=x[:, kc, ts_], rhs=wt[:, kc, cVA:cVA + 512],
                                                                     start=(kc == 0), stop=(kc == KC - 1)), R=[x, wt], W=[pv2])
                S.act(lambda e, pv2=pv2, tt=tt: e.activation(out=va[:, tt, :], in_=pv2[:], func=AF.Identity), R=[pv2], W=[va])
            for h in range(4):
                pr = nb()
                gemm_fm(pr, wt, cRA + h * 128, 128, x, G)
                S.act(lambda e, pr=pr, h=h: e.activation(out=silur[:, h, :], in_=pr[:], func=AF.Silu), R=[pr], W=[silur])
            pg = nb()
            gemm_fm(pg, wt, cGA, 16, x, G)
            S.act(lambda e, pg=pg: e.activation(out=gab[:, 0, :], in_=pg[0:16, :], func=AF.Identity), R=[pg], W=[gab])
            for h in range(4):
                pz = nb()
                S.pe(lambda e, pz=pz, h=h: e.matmul(pz[0:64, :], lhsT=gw[:, 0, h * 64:(h + 1) * 64], rhs=gab[:, 0, :],
                                                    start=True, stop=True), R=[gw, gab], W=[pz])
                S.act(lambda e, pz=pz, h=h: e.activation(out=ex[:], in_=pz[0:64, :], func=AF.Exp, scale=-1.0, bias=ngb[:, h:h + 1]),
                      R=[pz, ngb], W=[ex])
                S.act(lambda e: e.activation(out=ex[:], in_=ex[:], func=AF.Ln, bias=1.0), R=[ex], W=[ex])
                for c in range(8):
                    ccs = slice(c * 64, (c + 1) * 64)
                    S.dve(lambda e, ccs=ccs: e.tensor_tensor_scan(out=cum[:, ccs], data0=ones_col[0:64, :], data1=ex[:, ccs],
                                                                  initial=0.0, op0=ALU.mult, op1=ALU.add),
                          R=[ones_col, ex], W=[cum])
                ebh = eb[h]
                S.act(lambda e, ebh=ebh: e.activation(out=ebh[:], in_=cum[:], func=AF.Exp, scale=-1.0 / 16), R=[cum], W=[ebh])
                S.act(lambda e: e.activation(out=enb[:], in_=cum[:], func=AF.Exp, scale=1.0 / 16), R=[cum], W=[enb])
                pqa, pka = nb(), nb()
                gemm_fm(pqa, wt, cQA + h * 64, 64, x, G)
                gemm_fm(pka, wt, cKA + h * 64, 64, x, G)
                S.dve(lambda e, pqa=pqa, ebh=ebh, h=h: e.scalar_tensor_tensor(out=qdec[h][:], in0=pqa[0:64, :], scalar=0.125, in1=ebh[:],
                                                                             op0=ALU.mult, op1=ALU.mult), R=[pqa, ebh], W=[qdec[h]])
                S.dve(lambda e, pka=pka: e.tensor_tensor(out=kdf[:], in0=pka[0:64, :], in1=enb[:], op=ALU.mult), R=[pka, enb], W=[kdf])
                S.act(lambda e, h=h: e.activation(out=kdec[h][:], in_=kdf[:], func=AF.Identity), R=[kdf], W=[kdec[h]])
                for c in range(8):
                    ccs = slice(c * 64, (c + 1) * 64)
                    S.dve(lambda e, ccs=ccs, c=c, ebh=ebh: e.tensor_scalar(out=kend[:, ccs], in0=kdf[:, ccs],
                                                                          scalar1=ebh[:, c * 64 + 63:c * 64 + 64], scalar2=None,
                                                                          op0=ALU.mult), R=[kdf, ebh], W=[kend])
                for tt in range(4):
                    S.pe(lambda e, tt=tt: e.transpose(out=ptr[:, tt, 0:64], in_=kend[:, tt * 128:(tt + 1) * 128],
                                                      identity=ident_b[0:64, 0:64]), R=[kend, ident_b], W=[ptr])
                S.dve(lambda e, h=h: e.tensor_copy(out=kendT[:, :, h, :], in_=ptr[:, :, 0:64]), R=[ptr], W=[kendT])
            if dbg and g == 0:
                for h in range(4):
                    for nm, tl, shp, dt_ in (("qdec", qdec[h], [64, G], BF16), ("kdec", kdec[h], [64, G], BF16), ("eb", eb[h], [64, G], F32)):
                        dd = nc.dram_tensor("dbg_%s%d_%d" % (nm, h, i), shp, dt_, kind="ExternalOutput").ap()
                        S.dma("sp", dd, tl[:], R=[tl])
                dd = nc.dram_tensor("dbg_va_%d" % i, [128, 4, 512], BF16, kind="ExternalOutput").ap()
                S.dma("sp", dd, va[:], R=[va])
                dd = nc.dram_tensor("dbg_kendT_%d" % i, [128, 4, 4, 64], BF16, kind="ExternalOutput").ap()
                S.dma("sp", dd, kendT[:], R=[kendT])
                dd = nc.dram_tensor("dbg_silur_%d" % i, [128, 4, G], F32, kind="ExternalOutput").ap()
                S.dma("sp", dd, silur[:], R=[silur])
            for h in range(4):
                ebh = eb[h]
                for tt in range(4):
                    ts_ = slice(tt * 128, (tt + 1) * 128)
                    S.pe(lambda e, h=h, ts_=ts_: e.matmul(pmisc[:, 0:128], lhsT=kdec[h][:, ts_], rhs=qdec[h][:, ts_], start=True, stop=True),
                         R=[kdec[h], qdec[h]], W=[pmisc])
                    am = atm[tt % 2]
                    S.dve(lambda e, am=am: e.tensor_tensor(out=am[:], in0=pmisc[:, 0:128], in1=gmask[:], op=ALU.mult),
                          R=[pmisc, gmask], W=[am])
                    S.pe(lambda e, am=am, h=h, tt=tt, ts_=ts_: e.matmul(po[:, ts_], lhsT=va[:, tt, h * 128:(h + 1) * 128], rhs=am[:],
                                                                     start=(tt == 0), stop=False), R=[va, am], W=[po])
                    for c2 in range(2):
                        c = tt * 2 + c2
                        ccs = slice(c * 64, (c + 1) * 64)
                        pb = c2 * 64
                        S.pe(lambda e, h=h, ccs=ccs: e.matmul(po[:, ccs], lhsT=Sb[h][:], rhs=qdec[h][:, ccs], start=False, stop=True),
                             R=[Sb[h], qdec[h]], W=[po])
                        S.pe(lambda e, h=h, tt=tt, pb=pb: e.matmul(pmisc[0:64, 256:384], lhsT=kendT[pb:pb + 64, tt, h, :],
                                                                   rhs=va[pb:pb + 64, tt, h * 128:(h + 1) * 128], start=True, stop=True),
                             R=[kendT, va], W=[pmisc])
                        S.dve(lambda e, h=h, c=c, ebh=ebh: e.scalar_tensor_tensor(out=Sf[h][:], in0=Sf[h][:],
                                                                                 scalar=ebh[:, c * 64 + 63:c * 64 + 64],
                                                                                 in1=pmisc[0:64, 256:384], op0=ALU.mult, op1=ALU.add),
                              R=[Sf[h], ebh, pmisc], W=[Sf[h]])
                        S.act(lambda e, h=h: e.activation(out=Sb[h][:], in_=Sf[h][:], func=AF.Identity), R=[Sf[h]], W=[Sb[h]])
                S.act(lambda e: e.activation(out=osq[:], in_=po[:], func=AF.Square), R=[po], W=[osq])
                S.pe(lambda e: e.matmul(pq[:], lhsT=ones128[:], rhs=osq[:], start=True, stop=True), R=[ones128, osq], W=[pq])
                rstd_from_msq(orstd, pq, eps_rms, 128, G)
                if dbg and g == 0:
                    dd = nc.dram_tensor("dbg_po%d_%d" % (h, i), [128, G], F32, kind="ExternalOutput").ap()
                    S.act(lambda e: e.activation(out=ot[:], in_=po[:], func=AF.Identity), R=[po], W=[ot])
                    S.dma("sp", dd, ot[:], R=[ot])
                S.dve(lambda e: e.tensor_tensor(out=ot[:], in0=po[:], in1=orstd[:], op=ALU.mult), R=[po, orstd], W=[ot])
                om = omix[h % 2]
                S.dve(lambda e, om=om, h=h: e.scalar_tensor_tensor(out=om[:], in0=ot[:], scalar=colap("gla_g", j), in1=silur[:, h, :],
                                                                  op0=ALU.mult, op1=ALU.mult), R=[ot, cols, silur], W=[om])
                S.dma("sp", mix_d[h * 128:(h + 1) * 128, gs], om[:], R=[om])
        st.close()

    def stage_m_diff(i):
        j = i // 2
        lam_init = 0.8 - 0.6 * float(np.exp(-0.3 * i))
        st = Stage()
        lv = [st.sb([128, 64], F32) for _ in range(4)]
        for a in range(4):
            S.dma("sp", lv[a][:], lamv[a][j:j + 1, :].partition_broadcast(128), W=[lv[a]])
        lt = st.sb([128, 64], F32)
        ls = [st.sb([128, 1], F32) for _ in range(2)]
        nlam = st.sb([128, 1], F32)
        gcol = st.sb([128, 1], F32)
        for a in range(2):
            S.dve(lambda e, a=a: e.tensor_tensor(out=lt[:], in0=lv[2 * a][:], in1=lv[2 * a + 1][:], op=ALU.mult),
                  R=[lv[2 * a], lv[2 * a + 1]], W=[lt])
            S.dve(lambda e, a=a: e.reduce_sum(out=ls[a][:], in_=lt[:], axis=mybir.AxisListType.X), R=[lt], W=[ls[a]])
            S.act(lambda e, a=a: e.activation(out=ls[a][:], in_=ls[a][:], func=AF.Exp), R=[ls[a]], W=[ls[a]])
        S.dve(lambda e: e.tensor_tensor(out=nlam[:], in0=ls[1][:], in1=ls[0][:], op=ALU.subtract), R=[ls[0], ls[1]], W=[nlam])
        S.dve(lambda e: e.tensor_scalar(out=nlam[:], in0=nlam[:], scalar1=-lam_init, scalar2=None, op0=ALU.add), R=[nlam], W=[nlam])
        S.dve(lambda e: e.tensor_scalar(out=gcol[:], in0=colap("diff_g", j), scalar1=1.0 - lam_init, scalar2=None, op0=ALU.mult),
              R=[cols], W=[gcol])
        k1 = st.sb([64, T], BF16)
        k2 = st.sb([64, T], BF16)
        vv = st.sb([128, NT, 128], BF16)
        q1 = [st.sb([64, G], BF16) for _ in range(2)]
        q2 = [st.sb([64, G], BF16) for _ in range(2)]
        pS = [(st.ps(), st.ps()) for _ in range(2)]
        pO = (st.ps(), st.ps())
        pL = (st.ps(), st.ps())
        pp = [(st.sb([128, G], BF16), st.sb([128, G], BF16)) for _ in range(2)]
        r1 = st.sb([128, G], F32)
        r2 = st.sb([128, G], F32)
        o_t = st.sb([128, G], F32)
        osq = st.sb([128, G], F32)
        orstd = st.sb([128, G], F32)
        omix = [st.sb([128, G], BF16) for _ in range(2)]
        itc = [0]
        for h in range(4):
            S.dma("sp", k1[:], kd_d[(2 * h) * 64:(2 * h + 1) * 64, :], W=[k1])
            S.dma("sp", k2[:], kd_d[(2 * h + 1) * 64:(2 * h + 2) * 64, :], W=[k2])
            for n0 in range(0, NT, 8):
                n1 = min(NT, n0 + 8)
                S.dma("sp", vv[:, n0:n1, :], vd_d.rearrange("(n p) f -> p n f", p=128)[:, n0:n1, h * 128:(h + 1) * 128], W=[vv])
            for g in range(NG):
                gs = slice(g * G, (g + 1) * G)
                qa, qb_ = q1[g % 2], q2[g % 2]
                S.dma("sp", qa[:], qd_d[(2 * h) * 64:(2 * h + 1) * 64, gs], W=[qa])
                S.dma("sp", qb_[:], qd_d[(2 * h + 1) * 64:(2 * h + 2) * 64, gs], W=[qb_])
                nkt = 4 * g + 4

                def qk(kt):
                    c0 = 0 if kt < 4 * g else 128 * (kt - 4 * g)
                    ks = slice(kt * 128, (kt + 1) * 128)
                    ps1, ps2 = pS[itc[0] % 2]
                    p1, p2 = pp[itc[0] % 2]
                    itc[0] += 1
                    for (psx, kx, qx, px) in ((ps1, k1, qa, p1), (ps2, k2, qb_, p2)):
                        S.pe(lambda e: e.matmul(psx[:, c0:G], lhsT=kx[:, ks], rhs=qx[:, c0:G], start=True, stop=True),
                             R=[kx, qx], W=[psx])
                        S.act(lambda e: e.activation(out=px[:, c0:G], in_=psx[:, c0:G], func=AF.Exp, scale=0.125),
                              R=[psx], W=[px])
                        if kt >= 4 * g:
                            S.dve(lambda e: e.memset(px[64:128, c0:c0 + 64], 0.0), R=[px], W=[px])
                    return (p1, p2, c0)

                def pv(kt, st_):
                    p1, p2, c0 = st_
                    for (px, pOx, pLx) in ((p1, pO[0], pL[0]), (p2, pO[1], pL[1])):
                        S.pe(lambda e: e.matmul(pOx[:, c0:G], lhsT=vv[:, kt, :], rhs=px[:, c0:G],
                                                start=(kt == 0), stop=(kt == nkt - 1)), R=[vv, px], W=[pOx])
                        S.pe(lambda e: e.matmul(pLx[:, c0:G], lhsT=ones_b[:], rhs=px[:, c0:G],
                                                start=(kt == 0), stop=(kt == nkt - 1)), R=[ones_b, px], W=[pLx])

                cur = qk(0)
                for kt in range(nkt):
                    nxt = qk(kt + 1) if kt + 1 < nkt else None
                    pv(kt, cur)
                    cur = nxt
                S.dve(lambda e: e.reciprocal(out=r1[:], in_=pL[0][:]), R=[pL[0]], W=[r1])
                S.dve(lambda e: e.reciprocal(out=r2[:], in_=pL[1][:]), R=[pL[1]], W=[r2])
                S.dve(lambda e: e.tensor_tensor(out=r1[:], in0=pO[0][:], in1=r1[:], op=ALU.mult), R=[pO[0], r1], W=[r1])
                S.dve(lambda e: e.tensor_tensor(out=r2[:], in0=pO[1][:], in1=r2[:], op=ALU.mult), R=[pO[1], r2], W=[r2])
                S.dve(lambda e: e.scalar_tensor_tensor(out=o_t[:], in0=r2[:], scalar=nlam[:], in1=r1[:], op0=ALU.mult, op1=ALU.add),
                      R=[r1, r2, nlam], W=[o_t])
                S.act(lambda e: e.activation(out=osq[:], in_=o_t[:], func=AF.Square), R=[o_t], W=[osq])
                pq = pS[itc[0] % 2][0]
                S.pe(lambda e, pq=pq: e.matmul(pq[:], lhsT=ones128[:], rhs=osq[:], start=True, stop=True), R=[ones128, osq], W=[pq])
                rstd_from_msq(orstd, pq, eps_rms, 128, G)
                S.dve(lambda e: e.tensor_tensor(out=o_t[:], in0=o_t[:], in1=orstd[:], op=ALU.mult), R=[o_t, orstd], W=[o_t])
                om = omix[g % 2]
                S.dve(lambda e, om=om: e.tensor_scalar(out=om[:], in0=o_t[:], scalar1=gcol[:], scalar2=None, op0=ALU.mult),
                      R=[o_t, gcol], W=[om])
                S.dma("sp", mix_d[512 + h * 128:512 + (h + 1) * 128, gs], om[:], R=[om])
        st.close()

    def stage_c_odd(i):
        j = i // 2
        st = Stage()
        w = odd_w_in[j]
        cQ, cQs, cK, cKs, cV = 0, 1024, 2048, 2048 + 256, 2048 + 512
        NW = 2048 + 512 + 128
        wt = st.sb([128, KC, NW], BF16)
        load_w(wt, cQ, w, D, 0, 1024)
        load_w_swapped(wt, cQs, w, D, 0, 16)
        for kv in range(2):
            for dup in range(2):
                load_w(wt, cK + kv * 128 + dup * 64, w, D, 1024 + kv * 64, 64)
                load_w_swapped(wt, cKs + kv * 128 + dup * 64, w, D, 1024 + kv * 64, 1)
        load_w(wt, cV, w, D, 1152, 128)
        sk = st.sb([128, 16], F32)
        S.dma("sp", sk[:], sinks_in[j:j + 1, :].partition_broadcast(128), W=[sk])
        S.act(lambda e: e.activation(out=sk[:], in_=sk[:], func=AF.Exp), R=[sk], W=[sk])
        NSB = 4
        banks = [st.ps() for _ in range(2)]
        pSb = [st.ps() for _ in range(NSB)]
        pOL = [(st.ps(), st.ps())] * 2
        bk = [0]

        def nb():
            b = banks[bk[0] % 2]
            bk[0] += 1
            return b
        xg = [st.sb([128, KC, G], BF16) for _ in range(2)]
        csg = [(st.sb([128, G], F32), st.sb([128, G], F32)) for _ in range(2)]
        t1 = [st.sb([128, G], F32) for _ in range(2)]
        t2 = [st.sb([128, G], F32) for _ in range(2)]
        qrot = st.sb([128, 8, G], BF16)
        kk = [[st.sb([128, 4, 128], BF16) for _ in range(2)] for _ in range(2)]
        vk = [st.sb([128, 4, 128], BF16) for _ in range(2)]
        pp = [st.sb([128, 256], BF16) for _ in range(NSB)]
        rrs = [st.sb([64, G], F32) for _ in range(2)]
        oh = [st.sb([64, G], BF16) for _ in range(2)]
        itc = [0]

        def load_g(g):
            gs = slice(g * G, (g + 1) * G)
            S.dma("sp", xg[g % 2][:], fm(xb_d)[:, :, gs], W=[xg[g % 2]])
            S.dma("sp", csg[g % 2][0][:], cos_d[:, gs], W=[csg[g % 2][0]])
            S.dma("sp", csg[g % 2][1][:], sin_d[:, gs], W=[csg[g % 2][1]])

        load_g(0)
        for g in range(NG):
            gs = slice(g * G, (g + 1) * G)
            if g + 1 < NG:
                load_g(g + 1)
            x = xg[g % 2]
            cs, sn = csg[g % 2]
            hf = g % 2
            for m in range(10):
                p1, p2 = nb(), nb()
                if m < 8:
                    gemm_fm(p1, wt, cQ + m * 128, 128, x, G)
                    gemm_fm(p2, wt, cQs + m * 128, 128, x, G)
                else:
                    gemm_fm(p1, wt, cK + (m - 8) * 128, 128, x, G)
                    gemm_fm(p2, wt, cKs + (m - 8) * 128, 128, x, G)
                a, b_ = t1[m % 2], t2[m % 2]
                S.dve(lambda e, a=a, p1=p1: e.tensor_tensor(out=a[:], in0=p1[:], in1=cs[:], op=ALU.mult), R=[p1, cs], W=[a])
                S.dve(lambda e, b_=b_, p2=p2: e.tensor_tensor(out=b_[:], in0=p2[:], in1=sn[:], op=ALU.mult), R=[p2, sn], W=[b_])
                if m < 8:
                    S.pool(lambda e, a=a, b_=b_, m=m: e.tensor_tensor(out=qrot[:, m, :], in0=a[:], in1=b_[:], op=ALU.add),
                           R=[a, b_], W=[qrot])
                else:
                    kt_ = kk[m - 8][hf]
                    S.pool(lambda e, a=a, b_=b_, kt_=kt_: e.tensor_tensor(out=kt_[:].rearrange("p a b -> p (a b)"), in0=a[:], in1=b_[:],
                                                                         op=ALU.add), R=[a, b_], W=[kt_])
            for tt in range(4):
                pv = nb()
                for kc in range(KC):
                    S.pe(lambda e, kc=kc, pv=pv, tt=tt: e.matmul(pv[:, 0:128], lhsT=x[:, kc, tt * 128:(tt + 1) * 128], rhs=wt[:, kc, cV:cV + 128],
                                                                 start=(kc == 0), stop=(kc == KC - 1)), R=[x, wt], W=[pv])
                S.act(lambda e, pv=pv, tt=tt: e.activation(out=vk[hf][:, tt, :], in_=pv[:, 0:128], func=AF.Identity), R=[pv], W=[vk[hf]])
            items = [(hq, s_) for hq in range(16) for s_ in range(5) if not (s_ == 0 and g == 0)]

            def qk(hq, s_):
                m, po_ = hq // 2, (hq % 2) * 64
                kv = hq // 8
                if s_ == 0:
                    ktile, vtile, sl = kk[kv][1 - hf], vk[1 - hf], 3
                else:
                    ktile, vtile, sl = kk[kv][hf], vk[hf], s_ - 1
                c0 = max(0, (s_ - 1) * 128)
                c1 = min(G, (s_ + 1) * 128)
                n = c1 - c0
                psx = pSb[itc[0] % NSB]
                px = pp[itc[0] % NSB]
                itc[0] += 1
                S.pe(lambda e: e.matmul(psx[:, 0:n], lhsT=ktile[po_:po_ + 64, sl, :], rhs=qrot[po_:po_ + 64, m, c0:c1],
                                        start=True, stop=True), R=[ktile, qrot], W=[psx])
                S.act(lambda e: e.activation(out=px[:, 0:n], in_=psx[:, 0:n], func=AF.Exp, scale=0.125), R=[psx], W=[px])
                if s_ >= 1:
                    S.dve(lambda e: e.memset(px[64:128, 0:64], 0.0), R=[px], W=[px])
                if s_ <= 3:
                    off = 128 if s_ >= 1 else 0
                    S.dve(lambda e: e.memset(px[0:64, off + 64:off + 128], 0.0), R=[px], W=[px])
                return (px, vtile, sl, c0, c1, n, kv)

            def pv(hq, s_, st_):
                px, vtile, sl, c0, c1, n, kv = st_
                pO, pL = pOL[hq % 2]
                first = (s_ == 0) or (s_ == 1 and g == 0)
                S.pe(lambda e: e.matmul(pO[0:64, c0:c1], lhsT=vtile[:, sl, kv * 64:(kv + 1) * 64], rhs=px[:, 0:n],
                                        start=first, stop=(s_ == 4)), R=[vtile, px], W=[pO])
                S.pe(lambda e: e.matmul(pL[0:64, c0:c1], lhsT=ones_b[:, 0:64], rhs=px[:, 0:n],
                                        start=first, stop=(s_ == 4)), R=[ones_b, px], W=[pL])
                if s_ == 4:
                    rr = rrs[hq % 2]
                    S.act(lambda e: e.activation(out=rr[:], in_=pL[0:64, :], func=AF.Identity, bias=sk[0:64, hq:hq + 1]),
                          R=[pL, sk], W=[rr])
                    S.dve(lambda e: e.reciprocal(out=rr[:], in_=rr[:]), R=[rr], W=[rr])
                    o = oh[hq % 2]
                    S.dve(lambda e: e.tensor_tensor(out=o[:], in0=pO[0:64, :], in1=rr[:], op=ALU.mult), R=[pO, rr], W=[o])
                    S.dma("sp", mix_d[hq * 64:(hq + 1) * 64, gs], o[:], R=[o])

            LA = NSB - 1
            pend = []
            nxt_i = 0
            for ii, (hq, s_) in enumerate(items):
                while nxt_i < len(items) and nxt_i <= ii + LA - 1:
                    pend.append(qk(*items[nxt_i]))
                    nxt_i += 1
                pv(hq, s_, pend.pop(0))
        st.close()

    def stage_a(i):
        j = i // 2
        st = Stage()
        w_o = (even_w_out if i % 2 == 0 else odd_w_out)[j]
        wo = st.sb([128, KC, D], BF16)
        wq = st.sb([128, KC, D], BF16)
        wm = st.sb([128, KC, D], BF16)
        mk = st.sb([128, KC, MEM], BF16)
        mv = st.sb([128, 2, D], BF16)
        banks = [st.ps() for _ in range(8)]
        pst = Stage()
        wkv = pst.sb([128, KC, 2 * D], BF16)
        memb = pst.sb([128, KC, MEM], BF16)
        load_w(wkv, 0, mem_w_kv[i], D, 0, 2 * D)
        for kc in range(KC):
            S.dma("pool", memb[:, kc, :], memT_in[kc * 128:(kc + 1) * 128, :], W=[memb])
        for m in range(8):
            p = banks[m % 4]
            gemm_fm(p, wkv, m * 128, 128, memb, MEM)
            S.act(lambda e, p=p, m=m: e.activation(out=mk[:, m, :], in_=p[:, 0:MEM], func=AF.Identity), R=[p], W=[mk])
        for mt in range(2):
            for n in range(2):
                p = banks[4 + (mt * 2 + n) % 4]
                for kc in range(KC):
                    S.pe(lambda e, p=p, kc=kc, mt=mt, n=n: e.matmul(p[:, :], lhsT=memb[:, kc, mt * 128:(mt + 1) * 128],
                                                                  rhs=wkv[:, kc, D + n * 512:D + (n + 1) * 512],
                                                                  start=(kc == 0), stop=(kc == KC - 1)), R=[memb, wkv], W=[p])
                S.act(lambda e, p=p, mt=mt, n=n: e.activation(out=mv[:, mt, n * 512:(n + 1) * 512], in_=p[:], func=AF.Identity),
                      R=[p], W=[mv])
        load_w(wo, 0, w_o, D, 0, D)
        load_w(wq, 0, mem_w_q[i], D, 0, D)
        load_w(wm, 0, mem_w_out[i], D, 0, D)
        S.flush()
        pst.es.close()
        mixg = [st.sb([128, KC, G], BF16) for _ in range(2)]
        xf = [st.sb([128, KC, G], F32) for _ in range(2)]
        xbt = st.sb([128, KC, G], BF16)
        qm = st.sb([128, KC, G], BF16)
        om = st.sb([128, KC, G], BF16)
        pmts = [[st.sb([128, G], BF16) for _ in range(2)] for _ in range(2)]
        itc = [0]
        rl = st.sb([128, G], F32)
        tmp = tuple(st.sb([128, G], F32) for _ in range(5))
        bk = [0]

        def nb():
            b = (banks[7], banks[3], banks[2])[bk[0] % 3]
            bk[0] += 1
            return b
        pS0, pS1, pO0, pO1, pLb = banks[0], banks[1], banks[4], banks[5], banks[6]

        def load_g(g):
            gs = slice(g * G, (g + 1) * G)
            S.dma("sp", mixg[g % 2][:], fm(mix_d)[:, :, gs], W=[mixg[g % 2]])
            S.dma("sp", xf[g % 2][:], fm(xres)[:, :, gs], W=[xf[g % 2]])

        load_g(0)
        for g in range(NG):
            gs = slice(g * G, (g + 1) * G)
            if g + 1 < NG:
                load_g(g + 1)
            mx, x = mixg[g % 2], xf[g % 2]
            if dbg:
                dt_ = tmp[0]
                for c in range(KC):
                    S.dve(lambda e, c=c: e.tensor_copy(out=dt_[:], in_=mx[:, c, :]), R=[mx], W=[dt_])
                    S.dma("sp", dbg_aps[(i, "mix")][c * 128:(c + 1) * 128, gs], dt_[:], R=[dt_])
            for m in range(KC):
                p = nb()
                gemm_fm(p, wo, m * 128, 128, mx, G)
                xm = x.sub(m)
                S.dve(lambda e: e.scalar_tensor_tensor(out=xm[:], in0=xm[:], scalar=ALPHA, in1=p[:],
                                                       op0=ALU.mult, op1=ALU.add), R=[xm, p], W=[xm])
            layer_norm(st, x, i, 0, pS0, pS1, x, xbt, tmp, G)
            for m in range(KC):
                p = nb()
                gemm_fm(p, wq, m * 128, 128, xbt, G)
                S.act(lambda e, p=p, m=m: e.activation(out=qm[:, m, :], in_=p[:], func=AF.Identity), R=[p], W=[qm])
            def sc(hd):
                pair = ((banks[0], banks[1]), (banks[2], banks[3]))[itc[0] % 2]
                pm = pmts[itc[0] % 2]
                itc[0] += 1
                for mt in range(2):
                    psx = pair[mt]
                    for jj in range(2):
                        S.pe(lambda e: e.matmul(psx[:], lhsT=mk[:, 2 * hd + jj, mt * 128:(mt + 1) * 128],
                                                rhs=qm[:, 2 * hd + jj, :], start=(jj == 0), stop=(jj == 1)), R=[mk, qm], W=[psx])
                    pmx = pm[mt]
                    S.act(lambda e: e.activation(out=pmx[:], in_=psx[:], func=AF.Exp, scale=1.0 / 16), R=[psx], W=[pmx])
                return pm

            def pvl(hd, pm):
                for mt in range(2):
                    pmx = pm[mt]
                    S.pe(lambda e: e.matmul(pLb[:], lhsT=ones_b[:], rhs=pmx[:], start=(mt == 0), stop=(mt == 1)),
                         R=[ones_b, pmx], W=[pLb])
                    for dc, pOx in ((0, pO0), (1, pO1)):
                        S.pe(lambda e: e.matmul(pOx[:], lhsT=mv[:, mt, hd * 256 + dc * 128:hd * 256 + (dc + 1) * 128],
                                                rhs=pmx[:], start=(mt == 0), stop=(mt == 1)), R=[mv, pmx], W=[pOx])
                S.dve(lambda e: e.reciprocal(out=rl[:], in_=pLb[:]), R=[pLb], W=[rl])
                for dc, pOx in ((0, pO0), (1, pO1)):
                    oc = om.sub(2 * hd + dc)
                    S.dve(lambda e: e.tensor_tensor(out=oc[:], in0=pOx[:], in1=rl[:], op=ALU.mult), R=[pOx, rl], W=[oc])

            cur = sc(0)
            for hd in range(4):
                nxt = sc(hd + 1) if hd + 1 < 4 else None
                pvl(hd, cur)
                cur = nxt
            for m in range(KC):
                p = nb()
                gemm_fm(p, wm, m * 128, 128, om, G)
                xm = x.sub(m)
                S.dve(lambda e: e.scalar_tensor_tensor(out=xm[:], in0=xm[:], scalar=ALPHA, in1=p[:],
                                                       op0=ALU.mult, op1=ALU.add), R=[xm, p], W=[xm])
            layer_norm(st, x, i, 1, pS0, pS1, x, xbt, tmp, G)
            S.dma("sp", fm(xres2)[:, :, gs], x[:], R=[x])
            S.dma("sp", fm(xb2_d)[:, :, gs], xbt[:], R=[xbt])
            if dbg:
                S.dma("sp", fm(dbg_aps[(i, "x2")])[:, :, gs], x[:], R=[x])
        st.close()

    def stage_b(i, last):
        st = Stage()
        win = st.sb([128, KC, 2 * DFF], BF16)
        wout = st.sb([128, NJ, D], BF16)
        wblk = {}
        for j0 in range(0, NJ, 2):
            for half in range(2):
                c0_ = half * DFF + j0 * 128
                v_ = Tl(win.ap[:, :, c0_:c0_ + 256])
                load_w(v_, 0, ffn_w_in[i], D, c0_, 256)
                wblk[(j0, half)] = v_
        load_w(wout, 0, ffn_w_out[i], DFF, 0, D)
        banks = [st.ps() for _ in range(8)]
        xb_t = [st.sb([128, KC, 2 + GB], BF16) for _ in range(2)]
        xf = [st.sb([128, KC, GB], F32) for _ in range(2)]
        xo_b = st.sb([128, KC, GB], BF16)
        hb = st.sb([128, NJ, GB], BF16)
        cv = [st.sb([128, GB], F32) for _ in range(4)]
        ge = [st.sb([128, GB], F32) for _ in range(4)]
        tmp = tuple(st.sb([128, GB], F32) for _ in range(5))
        bk = [0]

        def nb():
            b = banks[bk[0] % 8]
            bk[0] += 1
            return b

        def load_g(g):
            t = xb_t[g % 2]
            if g == 0:
                S.dve(lambda e: e.memset(t[:, :, 0:2], 0.0), W=[t])
                S.dma("sp", t[:, :, 2:2 + GB], fm(xb2_d)[:, :, 0:GB], W=[t])
            else:
                S.dma("sp", t[:], fm(xb2_d)[:, :, g * GB - 2:(g + 1) * GB], W=[t])
            S.dma("sp", xf[g % 2][:], fm(xres2)[:, :, g * GB:(g + 1) * GB], W=[xf[g % 2]])

        def cw(k, jj):
            return colap("conv_w", (i * 3 + k) * NJ + jj)

        load_g(0)
        for g in range(NGB):
            gs = slice(g * GB, (g + 1) * GB)
            if g + 1 < NGB:
                load_g(g + 1)
            xb_, x = xb_t[g % 2], xf[g % 2]
            for j0 in range(0, NJ, 2):
                pr_ = []
                for jj in (j0, j0 + 1):
                    pg, pu = nb(), nb()
                    gemm_fm(pg, wblk[(j0, 0)], (jj - j0) * 128, 128, xb_, GB + 2, xs=slice(0, GB + 2))
                    gemm_fm(pu, wblk[(j0, 1)], (jj - j0) * 128, 128, xb_, GB, xs=slice(2, GB + 2))
                    pr_.append((jj, pg, pu, cv[jj % 4], ge[jj % 4]))
                for (jj, pg, pu, c_, g_) in pr_:
                    S.act(lambda e: e.activation(out=c_[:], in_=pg[:, 2:2 + GB], func=AF.Identity,
                                                 scale=cw(2, jj), bias=colap("conv_b", i * NJ + jj)), R=[pg, cols], W=[c_])
                for (jj, pg, pu, c_, g_) in pr_:
                    S.dve(lambda e: e.scalar_tensor_tensor(out=c_[:], in0=pg[:, 1:1 + GB], scalar=cw(1, jj), in1=c_[:],
                                                           op0=ALU.mult, op1=ALU.add), R=[pg, c_, cols], W=[c_])
                for (jj, pg, pu, c_, g_) in pr_:
                    S.dve(lambda e: e.scalar_tensor_tensor(out=c_[:], in0=pg[:, 0:GB], scalar=cw(0, jj), in1=c_[:],
                                                           op0=ALU.mult, op1=ALU.add), R=[pg, c_, cols], W=[c_])
                for (jj, pg, pu, c_, g_) in pr_:
                    S.act(lambda e: e.activation(out=g_[:], in_=c_[:], func=AF.Gelu_apprx_tanh), R=[c_], W=[g_])
                for (jj, pg, pu, c_, g_) in pr_:
                    hj = hb.sub(jj)
                    S.dve(lambda e: e.tensor_tensor(out=hj[:], in0=pu[:, 0:GB], in1=g_[:], op=ALU.mult), R=[pu, g_], W=[hj])
            for m in range(KC):
                p = nb()
                for jj in range(NJ):
                    S.pe(lambda e: e.matmul(p[:, 0:GB], lhsT=wout[:, jj, m * 128:(m + 1) * 128], rhs=hb[:, jj, :],
                                            start=(jj == 0), stop=(jj == NJ - 1)), R=[wout, hb], W=[p])
                xm = x.sub(m)
                S.dve(lambda e: e.scalar_tensor_tensor(out=xm[:], in0=xm[:], scalar=ALPHA, in1=p[:, 0:GB],
                                                       op0=ALU.mult, op1=ALU.add), R=[xm, p], W=[xm])
            pM0, pM1 = nb(), nb()
            layer_norm(st, x, i, 2, pM0, pM1, x, xo_b, tmp, GB)
            if last:
                S.dma("sp", fm(out_ap)[:, :, gs], x[:], R=[x])
            else:
                S.dma("sp", fm(xres)[:, :, gs], x[:], R=[x])
                S.dma("sp", fm(xb_d)[:, :, gs], xo_b[:], R=[xo_b])
            if dbg:
                S.dma("sp", fm(dbg_aps[(i, "x3")])[:, :, gs], x[:], R=[x])
        st.close()

    for i in range(nlayers):
        if i % 2 == 0:
            stage_c_even(i)
            stage_m_diff(i)
        else:
            stage_c_odd(i)
        stage_a(i)
        stage_b(i, last=(i == nlayers - 1))
    cst.es.close()
    es0.close()
    return nc


W_NAMES = ["even_w_in", "even_w_out", "gla_gate_w", "diff_lam_q1", "diff_lam_k1", "diff_lam_q2", "diff_lam_k2",
           "odd_w_in", "odd_w_out", "swa_sinks", "mem_w_q", "mem_w_kv", "mem_w_out", "ffn_w_in", "ffn_w_out"]


def make_in_map(inp, b, cols):
    m = {
        "xT": np.ascontiguousarray(np.asarray(inp["x"][b], np.float32).T),
        "memT": np.ascontiguousarray(np.asarray(inp["mem"][b], np.float32).T),
        "pos": np.ascontiguousarray(np.asarray(inp["positions"][b], np.int32).reshape(1, -1)),
        "cols": cols,
    }
    for n in W_NAMES:
        m[n] = np.ascontiguousarray(np.asarray(inp[n], np.float32))
    return m


def kernel(**inputs):
    x = np.asarray(inputs["x"])
    B, T, _ = x.shape
    cols = pack_cols(inputs)
    nc = build(T)
    in_maps = [make_in_map(inputs, b % B, cols) for b in range(8)]
    res = run_bass_kernel_spmd(nc, in_maps, core_ids=list(range(8)))
    out = np.stack([np.asarray(res.results[b]["outT"], np.float32).T for b in range(B)], axis=0)
    return out.astype(np.float32)
```

```python
import numpy as np
from contextlib import ExitStack
import concourse.bass as bass
import concourse.mybir as mybir
from concourse.bass_utils import run_bass_kernel_spmd

F32 = mybir.dt.float32
BF16 = mybir.dt.bfloat16
I32 = mybir.dt.int32
ALU = mybir.AluOpType
AF = mybir.ActivationFunctionType

D = 1024
KC = 8
DEPTH = 4
DFF = 2816
NJ = DFF // 128
MEM = 256
ALPHA = float((2 * DEPTH) ** 0.25)
G = 512
GB = 256

ENGS = ("pe", "act", "dve", "pool", "sp")
NS_DMA = 8


class Tl:
    __slots__ = ("ap", "key")
    _n = 0

    def __init__(self, ap, key=None):
        self.ap = ap
        if key is None:
            Tl._n += 1
            key = "t%d" % Tl._n
        self.key = key

    def __getitem__(self, idx):
        return self.ap[idx]

    def sub(self, c):
        return Tl(self.ap[:, c, :], (self.key, c))


class _Rec:
    def __getattr__(self, name):
        def f(*a, **k):
            self.call = (name, a, k)
            return self
        return f


class Sched:
    def __init__(self, nc, es):
        self.nc = nc
        self.csem = {e: es.enter_context(nc.semaphore("c_" + e)) for e in ("pe", "act", "dve", "pool")}
        self.dsem = {q: [es.enter_context(nc.semaphore("d_%s%d" % (q, i))) for i in range(NS_DMA)]
                     for q in ("sp", "act", "pool")}
        self.ccount = {e: 0 for e in self.csem}
        self.dcount = {q: 0 for q in self.dsem}
        self._reset()

    def _reset(self):
        self.ops = {e: [] for e in ENGS}
        self.lastw = {}
        self.readers = {}
        self.kids = {}

    def _ov(self, k):
        if isinstance(k, tuple):
            self.kids.setdefault(k[0], set()).add(k)
            return (k, k[0])
        ch = self.kids.get(k)
        return (k,) + tuple(ch) if ch else (k,)

    def op(self, eng, fn, R=(), W=(), dma=False):
        ops = self.ops[eng]
        idx = len(ops)
        me = (eng, idx, dma)
        deps = []
        for r in R:
            for k in self._ov(r.key):
                deps.extend(self.lastw.get(k, ()))
        for w in W:
            for k in self._ov(w.key):
                for lw in self.lastw.get(k, ()):
                    if (lw[0] != eng or lw[2] or dma) and not (lw[2] and dma):
                        deps.append(lw)
                for rd in self.readers.get(k, ()):
                    if rd[0] != eng or rd[2] or dma:
                        deps.append(rd)
        pr = _Rec()
        fn(pr)
        rec = {"call": pr.call, "deps": deps, "dma": dma, "sig": False}
        if dma:
            rec["dn"] = self.dcount[eng]
            self.dcount[eng] += 1
        ops.append(rec)
        for r in R:
            self.readers.setdefault(r.key, []).append(me)
        for w in W:
            prev = self.lastw.get(w.key, [])
            if dma and prev and all(p[2] for p in prev) and not self.readers.get(w.key):
                self.lastw[w.key] = prev + [me]
            else:
                self.lastw[w.key] = [me]
            self.readers[w.key] = []
        return me

    def pe(self, fn, R=(), W=()):
        return self.op("pe", fn, R, W)

    def act(self, fn, R=(), W=()):
        return self.op("act", fn, R, W)

    def dve(self, fn, R=(), W=()):
        return self.op("dve", fn, R, W)

    def pool(self, fn, R=(), W=()):
        return self.op("pool", fn, R, W)

    def dma(self, q, out, in_, R=(), W=(), **kw):
        return self.op(q, lambda e: e.dma_start(out=out, in_=in_, **kw), R, W, dma=True)

    def flush(self):
        nc = self.nc
        ops = self.ops
        for e in ENGS:
            for rec in ops[e]:
                for (de, di, dd) in rec["deps"]:
                    if not dd:
                        ops[de][di]["sig"] = True
        for e in ("pe", "act", "dve", "pool"):
            c = self.ccount[e]
            for rec in ops[e]:
                if rec["sig"] and not rec["dma"]:
                    c += 1
                    rec["sv"] = c
            self.ccount[e] = c
        dsem, csem = self.dsem, self.csem

        def emit(ename, eng):
            waited_c, waited_d = {}, {}
            for rec in ops[ename]:
                for (de, di, dd) in rec["deps"]:
                    drec = ops[de][di]
                    if dd:
                        n = drec["dn"]
                        val = 16 * (n // NS_DMA + 1)
                        key = (de, n % NS_DMA)
                        if waited_d.get(key, 0) >= val:
                            continue
                        waited_d[key] = val
                        eng.wait_ge(dsem[de][n % NS_DMA], val)
                    else:
                        if waited_c.get(de, -1) >= di:
                            continue
                        waited_c[de] = di
                        eng.wait_ge(csem[de], drec["sv"])
                if rec["dma"]:
                    n = rec["dn"]
                    slot = n % NS_DMA
                    if n >= NS_DMA:
                        val = 16 * (n // NS_DMA)
                        if waited_d.get((ename, slot), 0) < val:
                            waited_d[(ename, slot)] = val
                            eng.wait_ge(dsem[ename][slot], val)
                    nm, a, k = rec["call"]
                    getattr(eng, nm)(*a, **k).then_inc(dsem[ename][slot], 16)
                else:
                    nm, a, k = rec["call"]
                    ins = getattr(eng, nm)(*a, **k)
                    if rec["sig"]:
                        ins.then_inc(csem[ename], 1)
            if ename in dsem:
                tot = self.dcount[ename]
                for slot in range(NS_DMA):
                    cnt = (tot - slot + NS_DMA - 1) // NS_DMA if tot > slot else 0
                    if cnt > 0 and waited_d.get((ename, slot), 0) < 16 * cnt:
                        eng.wait_ge(dsem[ename][slot], 16 * cnt)

        with nc.Block() as block:
            @block.sync
            def _(e):
                emit("sp", e)

            @block.tensor
            def _(e):
                emit("pe", e)

            @block.scalar
            def _(e):
                emit("act", e)

            @block.vector
            def _(e):
                emit("dve", e)

            @block.gpsimd
            def _(e):
                emit("pool", e)
        self._reset()

    def clear_sems(self):
        allsems = list(self.csem.values()) + [s for v in self.dsem.values() for s in v]
        with self.nc.Block() as block:
            @block.sync
            def _(e):
                for s in allsems:
                    e.sem_clear(s)


def _col_layout():
    off = {}
    n = 0

    def add(name, cnt):
        nonlocal n
        off[name] = n
        n += cnt
    add("ln_g", DEPTH * 3 * KC)
    add("ln_b", DEPTH * 3 * KC)
    add("gate_b", 2 * 4)
    add("gla_g", 2)
    add("diff_g", 2)
    add("conv_w", DEPTH * 3 * NJ)
    add("conv_b", DEPTH * NJ)
    add("invf", 1)
    add("sign", 1)
    return off, n


COL, NCOL = _col_layout()


def pack_cols(inp):
    c = np.zeros((128, NCOL), np.float32)
    lg = np.asarray(inp["ln_g"], np.float32).reshape(DEPTH * 3, KC, 128)
    lb = np.asarray(inp["ln_b"], np.float32).reshape(DEPTH * 3, KC, 128)
    c[:, COL["ln_g"]:COL["ln_g"] + DEPTH * 3 * KC] = lg.reshape(-1, 128).T
    c[:, COL["ln_b"]:COL["ln_b"] + DEPTH * 3 * KC] = lb.reshape(-1, 128).T
    gb = np.asarray(inp["gla_gate_b"], np.float32).reshape(2 * 4, 64)
    c[:64, COL["gate_b"]:COL["gate_b"] + 8] = gb.T
    c[:, COL["gla_g"]:COL["gla_g"] + 2] = np.asarray(inp["gla_norm_g"], np.float32).T
    c[:, COL["diff_g"]:COL["diff_g"] + 2] = np.asarray(inp["diff_norm_g"], np.float32).T
    cw = np.asarray(inp["ffn_conv_w"], np.float32).reshape(DEPTH * 3 * NJ, 128)
    c[:, COL["conv_w"]:COL["conv_w"] + DEPTH * 3 * NJ] = cw.T
    cb = np.asarray(inp["ffn_conv_b"], np.float32).reshape(DEPTH * NJ, 128)
    c[:, COL["conv_b"]:COL["conv_b"] + DEPTH * NJ] = cb.T
    p = np.arange(128)
    c[:, COL["invf"]] = (10000.0 ** (-(np.arange(0, 64, 2, dtype=np.float32)) / 64.0)).astype(np.float32)[p % 32]
    c[:, COL["sign"]] = np.where((p % 64) < 32, -1.0, 1.0)
    return c


def build(T, dbg=False, nlayers=DEPTH):
    NLW = nlayers if dbg else DEPTH
    NE, NO = (NLW + 1) // 2, max(1, NLW // 2)
    NG = T // G
    NGB = T // GB
    NT = T // 128
    nc = bass.Bass("TRN2", target_bir_lowering=False)

    def din(name, shape, dt=F32):
        return nc.dram_tensor(name, list(shape), dt, kind="ExternalInput").ap()

    def dscr(name, shape, dt):
        return nc.dram_tensor(name, list(shape), dt, kind=("ExternalOutput" if dbg else "Internal")).ap()

    xT_in = din("xT", [D, T])
    memT_in = din("memT", [D, MEM])
    pos_in = din("pos", [1, T], I32)
    cols_in = din("cols", [128, NCOL])
    even_w_in = din("even_w_in", [NE, D, 3088])
    even_w_out = din("even_w_out", [NE, D, D])
    gate_w_in = din("gla_gate_w", [NE, 16, 256])
    lamv = [din(n, [NE, 64]) for n in ("diff_lam_q1", "diff_lam_k1", "diff_lam_q2", "diff_lam_k2")]
    odd_w_in = din("odd_w_in", [NO, D, 1280])
    odd_w_out = din("odd_w_out", [NO, D, D])
    sinks_in = din("swa_sinks", [NO, 16])
    mem_w_q = din("mem_w_q", [NLW, D, D])
    mem_w_kv = din("mem_w_kv", [NLW, D, 2 * D])
    mem_w_out = din("mem_w_out", [NLW, D, D])
    ffn_w_in = din("ffn_w_in", [NLW, D, 2 * DFF])
    ffn_w_out = din("ffn_w_out", [NLW, DFF, D])
    out_ap = nc.dram_tensor("outT", [D, T], F32, kind="ExternalOutput").ap()
    dbg_aps = {}
    if dbg:
        for i in range(nlayers):
            for s in ("mix", "x2", "x3"):
                dbg_aps[(i, s)] = nc.dram_tensor("dbg_%d_%s" % (i, s), [D, T], F32, kind="ExternalOutput").ap()

    xres = dscr("xres", [D, T], F32)
    xres2 = dscr("xres2", [D, T], F32)
    xb_d = dscr("xb", [D, T], BF16)
    xb2_d = dscr("xb2", [D, T], BF16)
    mix_d = dscr("mix", [D, T], BF16)
    qd_d = dscr("qd", [512, T], BF16)
    kd_d = dscr("kd", [512, T], BF16)
    vd_d = dscr("vd", [T, 512], BF16)
    cos_d = dscr("cosT", [128, T], F32)
    sin_d = dscr("sinT", [128, T], F32)

    def fm(ap):
        return ap.rearrange("(c p) t -> p c t", p=128)

    es0 = ExitStack()
    S = Sched(nc, es0)
    S.clear_sems()

    class Stage:
        _n = 0

        def __init__(self):
            self.es = ExitStack()

        def sb(self, shape, dt, key=None):
            Stage._n += 1
            return Tl(self.es.enter_context(nc.sbuf_tensor("sb%d" % Stage._n, list(shape), dt)), key)

        def ps(self, shape=(128, 512), dt=F32):
            Stage._n += 1
            return Tl(self.es.enter_context(nc.psum_tensor("ps%d" % Stage._n, list(shape), dt)))

        def close(self):
            S.flush()
            self.es.close()

    def load_w(wt, c_dst, w_ap, K, c0, n):
        for kc in range(K // 128):
            o = 0
            while o < n:
                m = min(2048, n - o)
                S.dma("pool", wt[:, kc, c_dst + o:c_dst + o + m],
                      w_ap[kc * 128:(kc + 1) * 128, c0 + o:c0 + o + m], W=[wt])
                o += m

    def load_w_swapped(wt, c_dst, w_ap, K, c0, nhm):
        for kc in range(K // 128):
            src = w_ap[kc * 128:(kc + 1) * 128, c0:c0 + nhm * 64].rearrange("p (h t f) -> p h t f", t=2, f=32)
            dst = wt[:, kc, c_dst:c_dst + nhm * 64].rearrange("p (h t f) -> p h t f", t=2, f=32)
            S.dma("pool", dst[:, :, 0, :], src[:, :, 1, :], W=[wt])
            S.dma("pool", dst[:, :, 1, :], src[:, :, 0, :], W=[wt])

    def gemm_fm(ps_t, wt, c0, m, xt, ncols, xs=slice(None), kcs=KC):
        for kc in range(kcs):
            S.pe(lambda e, kc=kc: e.matmul(ps_t[0:m, 0:ncols], lhsT=wt[:, kc, c0:c0 + m], rhs=xt[:, kc, xs],
                                           start=(kc == 0), stop=(kc == kcs - 1)), R=[wt, xt], W=[ps_t])

    cst = Stage()
    cols = cst.sb([128, NCOL], F32)
    ones_b = cst.sb([128, 128], BF16)
    onesD = cst.sb([128, 128], F32)
    ones128 = cst.sb([128, 128], F32)
    ident_b = cst.sb([128, 128], BF16)
    ident_f = cst.sb([128, 128], F32)
    gmask = cst.sb([128, 128], F32)
    eps_ln = cst.sb([128, 1], F32)
    eps_rms = cst.sb([128, 1], F32)
    ones_col = cst.sb([128, 64], F32)

    def colap(name, idx=0, rows=128):
        c = COL[name] + idx
        return cols[0:rows, c:c + 1]

    S.dma("sp", cols[:], cols_in, W=[cols])
    S.dve(lambda e: e.memset(ones_b[:], 1.0), W=[ones_b])
    S.dve(lambda e: e.memset(onesD[:], 1.0 / D), W=[onesD])
    S.dve(lambda e: e.memset(ones128[:], 1.0 / 128), W=[ones128])
    S.dve(lambda e: e.memset(eps_ln[:], 1e-5), W=[eps_ln])
    S.dve(lambda e: e.memset(eps_rms[:], 1e-6), W=[eps_rms])
    S.dve(lambda e: e.memset(ones_col[:], 1.0), W=[ones_col])
    S.dve(lambda e: e.memset(ident_f[:], 0.0), W=[ident_f])
    S.pool(lambda e: e.affine_select(out=ident_f[:], in_=ident_f[:], pattern=[[-1, 128]], compare_op=ALU.not_equal,
                                     fill=1.0, base=0, channel_multiplier=1), R=[ident_f], W=[ident_f])
    S.dve(lambda e: e.tensor_copy(out=ident_b[:], in_=ident_f[:]), R=[ident_f], W=[ident_b])
    S.dve(lambda e: e.memset(gmask[:], 1.0), W=[gmask])
    S.pool(lambda e: e.affine_select(out=gmask[:], in_=gmask[:], pattern=[[1, 128]], compare_op=ALU.is_ge,
                                     fill=0.0, base=0, channel_multiplier=-1), R=[gmask], W=[gmask])
    S.pool(lambda e: e.affine_select(out=gmask[:, 64:128], in_=gmask[:, 64:128], pattern=[[0, 64]], compare_op=ALU.is_ge,
                                     fill=0.0, base=-64, channel_multiplier=1), R=[gmask], W=[gmask])

    st = Stage()
    TWO_PI = float(2 * np.pi)
    C1 = 6.28125
    C2 = float(2 * np.pi - 6.28125)
    s0t = []
    for _ in range(2):
        d_ = {"pi": st.sb([128, G], I32), "ang": st.sb([128, G], F32), "xt": st.sb([128, KC, G], F32)}
        for which in ("cos", "sin"):
            d_["kf" + which] = st.sb([128, G], F32)
            d_["ki" + which] = st.sb([128, G], I32)
            d_["r" + which] = st.sb([128, G], F32)
        s0t.append(d_)
    for g in range(NG):
        gs = slice(g * G, (g + 1) * G)
        pi_t, ang = s0t[g % 2]["pi"], s0t[g % 2]["ang"]
        S.dma("sp", pi_t[:], pos_in[:, gs].partition_broadcast(128), W=[pi_t])
        S.dve(lambda e, a=ang, p=pi_t: e.tensor_copy(out=a[:], in_=p[:]), R=[pi_t], W=[ang])
        S.dve(lambda e, a=ang: e.tensor_scalar(out=a[:], in0=a[:], scalar1=colap("invf"), scalar2=None, op0=ALU.mult),
              R=[ang, cols], W=[ang])
        for which, dst in (("cos", cos_d), ("sin", sin_d)):
            sh = 0.25 if which == "cos" else 0.0
            kf, ki, r = s0t[g % 2]["kf" + which], s0t[g % 2]["ki" + which], s0t[g % 2]["r" + which]
            S.dve(lambda e, kf=kf, a=ang, sh=sh: e.tensor_scalar(out=kf[:], in0=a[:], scalar1=1.0 / TWO_PI, scalar2=sh,
                                                              op0=ALU.mult, op1=ALU.add), R=[ang], W=[kf])
            S.dve(lambda e, kf=kf, ki=ki: e.tensor_copy(out=ki[:], in_=kf[:]), R=[kf], W=[ki])
            S.dve(lambda e, kf=kf, ki=ki: e.tensor_copy(out=kf[:], in_=ki[:]), R=[ki], W=[kf])
            S.dve(lambda e, kf=kf, a=ang, r=r: e.scalar_tensor_tensor(out=r[:], in0=kf[:], scalar=-C1, in1=a[:],
                                                                    op0=ALU.mult, op1=ALU.add), R=[kf, ang], W=[r])
            S.dve(lambda e, kf=kf, r=r: e.scalar_tensor_tensor(out=r[:], in0=kf[:], scalar=-C2, in1=r[:],
                                                             op0=ALU.mult, op1=ALU.add), R=[kf, r], W=[r])
            if which == "cos":
                S.dve(lambda e, r=r: e.tensor_scalar(out=r[:], in0=r[:], scalar1=float(np.pi / 2), scalar2=3.1415925,
                                                   op0=ALU.add, op1=ALU.min), R=[r], W=[r])
            else:
                S.dve(lambda e, r=r: e.tensor_scalar(out=r[:], in0=r[:], scalar1=3.1415925, scalar2=None,
                                                   op0=ALU.min), R=[r], W=[r])
            S.dve(lambda e, r=r: e.tensor_scalar(out=r[:], in0=r[:], scalar1=-3.1415925, scalar2=None, op0=ALU.max),
                  R=[r], W=[r])
            S.act(lambda e, r=r: e.activation(out=r[:], in_=r[:], func=AF.Sin), R=[r], W=[r])
            if which == "sin":
                S.dve(lambda e, r=r: e.tensor_scalar(out=r[:], in0=r[:], scalar1=colap("sign"), scalar2=None,
                                                   op0=ALU.mult), R=[r, cols], W=[r])
            S.dma("sp", dst[:, gs], r[:], R=[r])
        xt = s0t[g % 2]["xt"]
        S.dma("sp", xt[:], fm(xT_in)[:, :, gs], W=[xt])
        S.dma("sp", fm(xres)[:, :, gs], xt[:], R=[xt])
        S.dma("pool", fm(xb_d)[:, :, gs], xt[:], R=[xt])
    st.close()

    def layer_norm(st, v, i, j, ps_mean, ps_msq, out_f, out_b, tmp, N):
        sq0, mean_sb, rstd, nmr, sq1 = tmp
        for c in range(KC):
            sq = (sq0, sq1)[c % 2]
            vc = v.sub(c)
            S.act(lambda e: e.activation(out=sq[:, 0:N], in_=vc[:], func=AF.Square), R=[vc], W=[sq])
            S.pe(lambda e: e.matmul(ps_mean[:, 0:N], lhsT=onesD[:], rhs=vc[:], start=(c == 0), stop=(c == KC - 1)),
                 R=[onesD, vc], W=[ps_mean])
            S.pe(lambda e: e.matmul(ps_msq[:, 0:N], lhsT=onesD[:], rhs=sq[:, 0:N], start=(c == 0), stop=(c == KC - 1)),
                 R=[onesD, sq], W=[ps_msq])
        S.act(lambda e: e.activation(out=mean_sb[:, 0:N], in_=ps_mean[:, 0:N], func=AF.Identity), R=[ps_mean], W=[mean_sb])
        S.dve(lambda e: e.tensor_tensor(out=rstd[:, 0:N], in0=mean_sb[:, 0:N], in1=mean_sb[:, 0:N], op=ALU.mult),
              R=[mean_sb], W=[rstd])
        S.dve(lambda e: e.tensor_tensor(out=rstd[:, 0:N], in0=ps_msq[:, 0:N], in1=rstd[:, 0:N], op=ALU.subtract),
              R=[ps_msq, rstd], W=[rstd])
        S.act(lambda e: e.activation(out=rstd[:, 0:N], in_=rstd[:, 0:N], func=AF.Ln, bias=eps_ln[:]), R=[rstd, eps_ln], W=[rstd])
        S.act(lambda e: e.activation(out=rstd[:, 0:N], in_=rstd[:, 0:N], func=AF.Exp, scale=-0.5), R=[rstd], W=[rstd])
        S.dve(lambda e: e.scalar_tensor_tensor(out=nmr[:, 0:N], in0=mean_sb[:, 0:N], scalar=-1.0, in1=rstd[:, 0:N],
                                               op0=ALU.mult, op1=ALU.mult), R=[mean_sb, rstd], W=[nmr])
        gi = (i * 3 + j) * KC
        for c in range(KC):
            vc, oc = v.sub(c), out_f.sub(c)
            S.dve(lambda e: e.tensor_tensor(out=oc[:], in0=vc[:], in1=rstd[:, 0:N], op=ALU.mult), R=[vc, rstd], W=[oc])
        for c in range(KC):
            oc, ob = out_f.sub(c), out_b.sub(c)
            S.dve(lambda e: e.tensor_tensor(out=oc[:], in0=oc[:], in1=nmr[:, 0:N], op=ALU.add), R=[oc, nmr], W=[oc])
            S.act(lambda e: e.activation(out=ob[:], in_=oc[:], func=AF.Identity,
                                         scale=colap("ln_g", gi + c), bias=colap("ln_b", gi + c)), R=[oc, cols], W=[ob])
            S.pool(lambda e: e.tensor_scalar(out=oc[:], in0=oc[:], scalar1=colap("ln_g", gi + c), scalar2=colap("ln_b", gi + c),
                                             op0=ALU.mult, op1=ALU.add), R=[oc, cols], W=[oc])

    def rstd_from_msq(dst, src_ps, eps_t, rows, N):
        S.act(lambda e: e.activation(out=dst[0:rows, 0:N], in_=src_ps[0:rows, 0:N], func=AF.Ln, bias=eps_t[0:rows, :]),
              R=[src_ps, eps_t], W=[dst])
        S.act(lambda e: e.activation(out=dst[0:rows, 0:N], in_=dst[0:rows, 0:N], func=AF.Exp, scale=-0.5), R=[dst], W=[dst])

    def stage_c_even(i):
        j = i // 2
        st = Stage()
        w = even_w_in[j]
        cQA, cKA, cRA, cGA, cVA, cVB, cQB, cQBs, cKB, cKBs = 0, 256, 512, 1024, 1040, 1552, 2064, 2576, 3088, 3600
        NW = 4112
        wt = st.sb([128, KC, NW], BF16)
        load_w(wt, cQA, w, D, 0, 256)
        load_w(wt, cKA, w, D, 256, 256)
        load_w(wt, cVA, w, D, 512, 512)
        load_w(wt, cRA, w, D, 1024, 512)
        load_w(wt, cGA, w, D, 1536, 16)
        load_w(wt, cQB, w, D, 1552, 512)
        load_w(wt, cKB, w, D, 2064, 512)
        load_w(wt, cVB, w, D, 2576, 512)
        load_w_swapped(wt, cQBs, w, D, 1552, 8)
        load_w_swapped(wt, cKBs, w, D, 2064, 8)
        gw = st.sb([16, 1, 256], BF16)
        S.dma("pool", gw[:, 0, :], gate_w_in[j], W=[gw])
        ngb = st.sb([64, 4], F32)
        S.dve(lambda e: e.tensor_scalar(out=ngb[:], in0=cols[0:64, COL["gate_b"] + 4 * j:COL["gate_b"] + 4 * j + 4],
                                        scalar1=-1.0, scalar2=None, op0=ALU.mult), R=[cols], W=[ngb])
        banks = [st.ps() for _ in range(6)]
        pmisc = st.ps()
        ptr = st.ps([128, 4, 128], BF16)
        Sf = [st.sb([64, 128], F32) for _ in range(4)]
        Sb = [st.sb([64, 128], BF16) for _ in range(4)]
        for h in range(4):
            S.dve(lambda e, h=h: e.memset(Sf[h][:], 0.0), W=[Sf[h]])
            S.dve(lambda e, h=h: e.memset(Sb[h][:], 0.0), W=[Sb[h]])
        xg = [st.sb([128, KC, G], BF16) for _ in range(2)]
        csg = [(st.sb([128, G], F32), st.sb([128, G], F32)) for _ in range(2)]
        t1 = [st.sb([128, G], F32) for _ in range(2)]
        t2 = [st.sb([128, G], F32) for _ in range(2)]
        qkb = [st.sb([128, G], BF16) for _ in range(2)]
        vtok = [st.sb([128, 512], BF16) for _ in range(2)]
        va = st.sb([128, 4, 512], BF16)
        silur = st.sb([128, 4, G], F32)
        gab = st.sb([16, 1, G], BF16)
        ex = st.sb([64, G], F32)
        cum = st.sb([64, G], F32)
        eb = [st.sb([64, G], F32) for _ in range(4)]
        enb = st.sb([64, G], F32)
        kdf = st.sb([64, G], F32)
        qdec = [st.sb([64, G], BF16) for _ in range(4)]
        kdec = [st.sb([64, G], BF16) for _ in range(4)]
        kend = st.sb([64, G], BF16)
        kendT = st.sb([128, 4, 4, 64], BF16)
        atm = [st.sb([128, 128], BF16) for _ in range(2)]
        osq = st.sb([128, G], F32)
        orstd = st.sb([128, G], F32)
        ot = st.sb([128, G], F32)
        omix = [st.sb([128, G], BF16) for _ in range(2)]
        bk = [0]

        def nb():
            b = banks[bk[0] % 4]
            bk[0] += 1
            return b
        po = banks[4]
        pq = banks[5]

        def load_g(g):
            gs = slice(g * G, (g + 1) * G)
            S.dma("sp", xg[g % 2][:], fm(xb_d)[:, :, gs], W=[xg[g % 2]])
            S.dma("sp", csg[g % 2][0][:], cos_d[:, gs], W=[csg[g % 2][0]])
            S.dma("sp", csg[g % 2][1][:], sin_d[:, gs], W=[csg[g % 2][1]])

        load_g(0)
        for g in range(NG):
            gs = slice(g * G, (g + 1) * G)
            if g + 1 < NG:
                load_g(g + 1)
            x = xg[g % 2]
            cs, sn = csg[g % 2]
            for (c_n, c_s, dst) in ((cQB, cQBs, qd_d), (cKB, cKBs, kd_d)):
                for m in range(4):
                    p1, p2 = nb(), nb()
                    gemm_fm(p1, wt, c_n + m * 128, 128, x, G)
                    gemm_fm(p2, wt, c_s + m * 128, 128, x, G)
                    a, b_, o = t1[m % 2], t2[m % 2], qkb[m % 2]
                    S.dve(lambda e, a=a, p1=p1: e.tensor_tensor(out=a[:], in0=p1[:], in1=cs[:], op=ALU.mult), R=[p1, cs], W=[a])
                    S.dve(lambda e, b_=b_, p2=p2: e.tensor_tensor(out=b_[:], in0=p2[:], in1=sn[:], op=ALU.mult), R=[p2, sn], W=[b_])
                    S.pool(lambda e, a=a, b_=b_, o=o: e.tensor_tensor(out=o[:], in0=a[:], in1=b_[:], op=ALU.add), R=[a, b_], W=[o])
                    S.dma("sp", dst[m * 128:(m + 1) * 128, gs], o[:], R=[o])
            for tt in range(4):
                ts_ = slice(tt * 128, (tt + 1) * 128)
                pv = nb()
                for kc in range(KC):
                    S.pe(lambda e, kc=kc, pv=pv, ts_=ts_: e.matmul(pv[:, :], lhsT=x[:, kc, ts_], rhs=wt[:, kc, cVB:cVB + 512],
                                                                   start=(kc == 0), stop=(kc == KC - 1)), R=[x, wt], W=[pv])
                vt = vtok[tt % 2]
                S.act(lambda e, vt=vt, pv=pv: e.activation(out=vt[:], in_=pv[:], func=AF.Identity), R=[pv], W=[vt])
                S.dma("sp", vd_d[g * G + tt * 128:g * G + (tt + 1) * 128, :], vt[:], R=[vt])
                pv2 = nb()
                for kc in range(KC):
                    S.pe(lambda e, kc=kc, pv2=pv2, ts_=ts_: e.matmul(pv2[:, :], lhsT=x[:, kc, ts_], rhs=wt[:, kc, cVA:cVA + 512],
                                                                     start=(kc == 0), stop=(kc == KC - 1)), R=[x, wt], W=[pv2])
                S.act(lambda e, pv2=pv2, tt=tt: e.activation(out=va[:, tt, :], in_=pv2[:], func=AF.Identity), R=[pv2], W=[va])
            for h in range(4):
                pr = nb()
                gemm_fm(pr, wt, cRA + h * 128, 128, x, G)
                S.act(lambda e, pr=pr, h=h: e.activation(out=silur[:, h, :], in_=pr[:], func=AF.Silu), R=[pr], W=[silur])
            pg = nb()
            gemm_fm(pg, wt, cGA, 16, x, G)
            S.act(lambda e, pg=pg: e.activation(out=gab[:, 0, :], in_=pg[0:16, :], func=AF.Identity), R=[pg], W=[gab])
            for h in range(4):
                pz = nb()
                S.pe(lambda e, pz=pz, h=h: e.matmul(pz[0:64, :], lhsT=gw[:, 0, h * 64:(h + 1) * 64], rhs=gab[:, 0, :],
                                                    start=True, stop=True), R=[gw, gab], W=[pz])
                S.act(lambda e, pz=pz, h=h: e.activation(out=ex[:], in_=pz[0:64, :], func=AF.Exp, scale=-1.0, bias=ngb[:, h:h + 1]),
                      R=[pz, ngb], W=[ex])
                S.act(lambda e: e.activation(out=ex[:], in_=ex[:], func=AF.Ln, bias=1.0), R=[ex], W=[ex])
                for c in range(8):
                    ccs = slice(c * 64, (c + 1) * 64)
                    S.dve(lambda e, ccs=ccs: e.tensor_tensor_scan(out=cum[:, ccs], data0=ones_col[0:64, :], data1=ex[:, ccs],
                                                                  initial=0.0, op0=ALU.mult, op1=ALU.add),
                          R=[ones_col, ex], W=[cum])
                ebh = eb[h]
                S.act(lambda e, ebh=ebh: e.activation(out=ebh[:], in_=cum[:], func=AF.Exp, scale=-1.0 / 16), R=[cum], W=[ebh])
                S.act(lambda e: e.activation(out=enb[:], in_=cum[:], func=AF.Exp, scale=1.0 / 16), R=[cum], W=[enb])
                pqa, pka = nb(), nb()
                gemm_fm(pqa, wt, cQA + h * 64, 64, x, G)
                gemm_fm(pka, wt, cKA + h * 64, 64, x, G)
                S.dve(lambda e, pqa=pqa, ebh=ebh, h=h: e.scalar_tensor_tensor(out=qdec[h][:], in0=pqa[0:64, :], scalar=0.125, in1=ebh[:],
                                                                             op0=ALU.mult, op1=ALU.mult), R=[pqa, ebh], W=[qdec[h]])
                S.dve(lambda e, pka=pka: e.tensor_tensor(out=kdf[:], in0=pka[0:64, :], in1=enb[:], op=ALU.mult), R=[pka, enb], W=[kdf])
                S.act(lambda e, h=h: e.activation(out=kdec[h][:], in_=kdf[:], func=AF.Identity), R=[kdf], W=[kdec[h]])
                for c in range(8):
                    ccs = slice(c * 64, (c + 1) * 64)
                    S.dve(lambda e, ccs=ccs, c=c, ebh=ebh: e.tensor_scalar(out=kend[:, ccs], in0=kdf[:, ccs],
                                                                          scalar1=ebh[:, c * 64 + 63:c * 64 + 64], scalar2=None,
                                                                          op0=ALU.mult), R=[kdf, ebh], W=[kend])
                for tt in range(4):
                    S.pe(lambda e, tt=tt: e.transpose(out=ptr[:, tt, 0:64], in_=kend[:, tt * 128:(tt + 1) * 128],
                                                      identity=ident_b[0:64, 0:64]), R=[kend, ident_b], W=[ptr])
                S.dve(lambda e, h=h: e.tensor_copy(out=kendT[:, :, h, :], in_=ptr[:, :, 0:64]), R=[ptr], W=[kendT])
            if dbg and g == 0:
                for h in range(4):
                    for nm, tl, shp, dt_ in (("qdec", qdec[h], [64, G], BF16), ("kdec", kdec[h], [64, G], BF16), ("eb", eb[h], [64, G], F32)):
                        dd = nc.dram_tensor("dbg_%s%d_%d" % (nm, h, i), shp, dt_, kind="ExternalOutput").ap()
                        S.dma("sp", dd, tl[:], R=[tl])
                dd = nc.dram_tensor("dbg_va_%d" % i, [128, 4, 512], BF16, kind="ExternalOutput").ap()
                S.dma("sp", dd, va[:], R=[va])
                dd = nc.dram_tensor("dbg_kendT_%d" % i, [128, 4, 4, 64], BF16, kind="ExternalOutput").ap()
                S.dma("sp", dd, kendT[:], R=[kendT])
                dd = nc.dram_tensor("dbg_silur_%d" % i, [128, 4, G], F32, kind="ExternalOutput").ap()
                S.dma("sp", dd, silur[:], R=[silur])
            for h in range(4):
                ebh = eb[h]
                for tt in range(4):
                    ts_ = slice(tt * 128, (tt + 1) * 128)
                    S.pe(lambda e, h=h, ts_=ts_: e.matmul(pmisc[:, 0:128], lhsT=kdec[h][:, ts_], rhs=qdec[h][:, ts_], start=True, stop=True),
                         R=[kdec[h], qdec[h]], W=[pmisc])
                    am = atm[tt % 2]
                    S.dve(lambda e, am=am: e.tensor_tensor(out=am[:], in0=pmisc[:, 0:128], in1=gmask[:], op=ALU.mult),
                          R=[pmisc, gmask], W=[am])
                    S.pe(lambda e, am=am, h=h, tt=tt, ts_=ts_: e.matmul(po[:, ts_], lhsT=va[:, tt, h * 128:(h + 1) * 128], rhs=am[:],
                                                                     start=(tt == 0), stop=False), R=[va, am], W=[po])
                    for c2 in range(2):
                        c = tt * 2 + c2
                        ccs = slice(c * 64, (c + 1) * 64)
                        pb = c2 * 64
                        S.pe(lambda e, h=h, ccs=ccs: e.matmul(po[:, ccs], lhsT=Sb[h][:], rhs=qdec[h][:, ccs], start=False, stop=True),
                             R=[Sb[h], qdec[h]], W=[po])
                        S.pe(lambda e, h=h, tt=tt, pb=pb: e.matmul(pmisc[0:64, 256:384], lhsT=kendT[pb:pb + 64, tt, h, :],
                                                                   rhs=va[pb:pb + 64, tt, h * 128:(h + 1) * 128], start=True, stop=True),
                             R=[kendT, va], W=[pmisc])
                        S.dve(lambda e, h=h, c=c, ebh=ebh: e.scalar_tensor_tensor(out=Sf[h][:], in0=Sf[h][:],
                                                                                 scalar=ebh[:, c * 64 + 63:c * 64 + 64],
                                                                                 in1=pmisc[0:64, 256:384], op0=ALU.mult, op1=ALU.add),
                              R=[Sf[h], ebh, pmisc], W=[Sf[h]])
                        S.act(lambda e, h=h: e.activation(out=Sb[h][:], in_=Sf[h][:], func=AF.Identity), R=[Sf[h]], W=[Sb[h]])
                S.act(lambda e: e.activation(out=osq[:], in_=po[:], func=AF.Square), R=[po], W=[osq])
                S.pe(lambda e: e.matmul(pq[:], lhsT=ones128[:], rhs=osq[:], start=True, stop=True), R=[ones128, osq], W=[pq])
                rstd_from_msq(orstd, pq, eps_rms, 128, G)
                if dbg and g == 0:
                    dd = nc.dram_tensor("dbg_po%d_%d" % (h, i), [128, G], F32, kind="ExternalOutput").ap()
                    S.act(lambda e: e.activation(out=ot[:], in_=po[:], func=AF.Identity), R=[po], W=[ot])
                    S.dma("sp", dd, ot[:], R=[ot])
                S.dve(lambda e: e.tensor_tensor(out=ot[:], in0=po[:], in1=orstd[:], op=ALU.mult), R=[po, orstd], W=[ot])
                om = omix[h % 2]
                S.dve(lambda e, om=om, h=h: e.scalar_tensor_tensor(out=om[:], in0=ot[:], scalar=colap("gla_g", j), in1=silur[:, h, :],
                                                                  op0=ALU.mult, op1=ALU.mult), R=[ot, cols, silur], W=[om])
                S.dma("sp", mix_d[h * 128:(h + 1) * 128, gs], om[:], R=[om])
        st.close()

    def stage_m_diff(i):
        j = i // 2
        lam_init = 0.8 - 0.6 * float(np.exp(-0.3 * i))
        st = Stage()
        lv = [st.sb([128, 64], F32) for _ in range(4)]
        for a in range(4):
            S.dma("sp", lv[a][:], lamv[a][j:j + 1, :].partition_broadcast(128), W=[lv[a]])
        lt = st.sb([128, 64], F32)
        ls = [st.sb([128, 1], F32) for _ in range(2)]
        nlam = st.sb([128, 1], F32)
        gcol = st.sb([128, 1], F32)
        for a in range(2):
            S.dve(lambda e, a=a: e.tensor_tensor(out=lt[:], in0=lv[2 * a][:], in1=lv[2 * a + 1][:], op=ALU.mult),
                  R=[lv[2 * a], lv[2 * a + 1]], W=[lt])
            S.dve(lambda e, a=a: e.reduce_sum(out=ls[a][:], in_=lt[:], axis=mybir.AxisListType.X), R=[lt], W=[ls[a]])
            S.act(lambda e, a=a: e.activation(out=ls[a][:], in_=ls[a][:], func=AF.Exp), R=[ls[a]], W=[ls[a]])
        S.dve(lambda e: e.tensor_tensor(out=nlam[:], in0=ls[1][:], in1=ls[0][:], op=ALU.subtract), R=[ls[0], ls[1]], W=[nlam])
        S.dve(lambda e: e.tensor_scalar(out=nlam[:], in0=nlam[:], scalar1=-lam_init, scalar2=None, op0=ALU.add), R=[nlam], W=[nlam])
        S.dve(lambda e: e.tensor_scalar(out=gcol[:], in0=colap("diff_g", j), scalar1=1.0 - lam_init, scalar2=None, op0=ALU.mult),
              R=[cols], W=[gcol])
        k1 = st.sb([64, T], BF16)
        k2 = st.sb([64, T], BF16)
        vv = st.sb([128, NT, 128], BF16)
        q1 = [st.sb([64, G], BF16) for _ in range(2)]
        q2 = [st.sb([64, G], BF16) for _ in range(2)]
        pS = [(st.ps(), st.ps()) for _ in range(2)]
        pO = (st.ps(), st.ps())
        pL = (st.ps(), st.ps())
        pp = [(st.sb([128, G], BF16), st.sb([128, G], BF16)) for _ in range(2)]
        r1 = st.sb([128, G], F32)
        r2 = st.sb([128, G], F32)
        o_t = st.sb([128, G], F32)
        osq = st.sb([128, G], F32)
        orstd = st.sb([128, G], F32)
        omix = [st.sb([128, G], BF16) for _ in range(2)]
        itc = [0]
        for h in range(4):
            S.dma("sp", k1[:], kd_d[(2 * h) * 64:(2 * h + 1) * 64, :], W=[k1])
            S.dma("sp", k2[:], kd_d[(2 * h + 1) * 64:(2 * h + 2) * 64, :], W=[k2])
            for n0 in range(0, NT, 8):
                n1 = min(NT, n0 + 8)
                S.dma("sp", vv[:, n0:n1, :], vd_d.rearrange("(n p) f -> p n f", p=128)[:, n0:n1, h * 128:(h + 1) * 128], W=[vv])
            for g in range(NG):
                gs = slice(g * G, (g + 1) * G)
                qa, qb_ = q1[g % 2], q2[g % 2]
                S.dma("sp", qa[:], qd_d[(2 * h) * 64:(2 * h + 1) * 64, gs], W=[qa])
                S.dma("sp", qb_[:], qd_d[(2 * h + 1) * 64:(2 * h + 2) * 64, gs], W=[qb_])
                nkt = 4 * g + 4

                def qk(kt):
                    c0 = 0 if kt < 4 * g else 128 * (kt - 4 * g)
                    ks = slice(kt * 128, (kt + 1) * 128)
                    ps1, ps2 = pS[itc[0] % 2]
                    p1, p2 = pp[itc[0] % 2]
                    itc[0] += 1
                    for (psx, kx, qx, px) in ((ps1, k1, qa, p1), (ps2, k2, qb_, p2)):
                        S.pe(lambda e: e.matmul(psx[:, c0:G], lhsT=kx[:, ks], rhs=qx[:, c0:G], start=True, stop=True),
                             R=[kx, qx], W=[psx])
                        S.act(lambda e: e.activation(out=px[:, c0:G], in_=psx[:, c0:G], func=AF.Exp, scale=0.125),
                              R=[psx], W=[px])
                        if kt >= 4 * g:
                            S.dve(lambda e: e.memset(px[64:128, c0:c0 + 64], 0.0), R=[px], W=[px])
                    return (p1, p2, c0)

                def pv(kt, st_):
                    p1, p2, c0 = st_
                    for (px, pOx, pLx) in ((p1, pO[0], pL[0]), (p2, pO[1], pL[1])):
                        S.pe(lambda e: e.matmul(pOx[:, c0:G], lhsT=vv[:, kt, :], rhs=px[:, c0:G],
                                                start=(kt == 0), stop=(kt == nkt - 1)), R=[vv, px], W=[pOx])
                        S.pe(lambda e: e.matmul(pLx[:, c0:G], lhsT=ones_b[:], rhs=px[:, c0:G],
                                                start=(kt == 0), stop=(kt == nkt - 1)), R=[ones_b, px], W=[pLx])

                cur = qk(0)
                for kt in range(nkt):
                    nxt = qk(kt + 1) if kt + 1 < nkt else None
                    pv(kt, cur)
                    cur = nxt
                S.dve(lambda e: e.reciprocal(out=r1[:], in_=pL[0][:]), R=[pL[0]], W=[r1])
                S.dve(lambda e: e.reciprocal(out=r2[:], in_=pL[1][:]), R=[pL[1]], W=[r2])
                S.dve(lambda e: e.tensor_tensor(out=r1[:], in0=pO[0][:], in1=r1[:], op=ALU.mult), R=[pO[0], r1], W=[r1])
                S.dve(lambda e: e.tensor_tensor(out=r2[:], in0=pO[1][:], in1=r2[:], op=ALU.mult), R=[pO[1], r2], W=[r2])
                S.dve(lambda e: e.scalar_tensor_tensor(out=o_t[:], in0=r2[:], scalar=nlam[:], in1=r1[:], op0=ALU.mult, op1=ALU.add),
                      R=[r1, r2, nlam], W=[o_t])
                S.act(lambda e: e.activation(out=osq[:], in_=o_t[:], func=AF.Square), R=[o_t], W=[osq])
                pq = pS[itc[0] % 2][0]
                S.pe(lambda e, pq=pq: e.matmul(pq[:], lhsT=ones128[:], rhs=osq[:], start=True, stop=True), R=[ones128, osq], W=[pq])
                rstd_from_msq(orstd, pq, eps_rms, 128, G)
                S.dve(lambda e: e.tensor_tensor(out=o_t[:], in0=o_t[:], in1=orstd[:], op=ALU.mult), R=[o_t, orstd], W=[o_t])
                om = omix[g % 2]
                S.dve(lambda e, om=om: e.tensor_scalar(out=om[:], in0=o_t[:], scalar1=gcol[:], scalar2=None, op0=ALU.mult),
                      R=[o_t, gcol], W=[om])
                S.dma("sp", mix_d[512 + h * 128:512 + (h + 1) * 128, gs], om[:], R=[om])
        st.close()

    def stage_c_odd(i):
        j = i // 2
        st = Stage()
        w = odd_w_in[j]
        cQ, cQs, cK, cKs, cV = 0, 1024, 2048, 2048 + 256, 2048 + 512
        NW = 2048 + 512 + 128
        wt = st.sb([128, KC, NW], BF16)
        load_w(wt, cQ, w, D, 0, 1024)
        load_w_swapped(wt, cQs, w, D, 0, 16)
        for kv in range(2):
            for dup in range(2):
                load_w(wt, cK + kv * 128 + dup * 64, w, D, 1024 + kv * 64, 64)
                load_w_swapped(wt, cKs + kv * 128 + dup * 64, w, D, 1024 + kv * 64, 1)
        load_w(wt, cV, w, D, 1152, 128)
        sk = st.sb([128, 16], F32)
        S.dma("sp", sk[:], sinks_in[j:j + 1, :].partition_broadcast(128), W=[sk])
        S.act(lambda e: e.activation(out=sk[:], in_=sk[:], func=AF.Exp), R=[sk], W=[sk])
        NSB = 4
        banks = [st.ps() for _ in range(2)]
        pSb = [st.ps() for _ in range(NSB)]
        pOL = [(st.ps(), st.ps())] * 2
        bk = [0]

        def nb():
            b = banks[bk[0] % 2]
            bk[0] += 1
            return b
        xg = [st.sb([128, KC, G], BF16) for _ in range(2)]
        csg = [(st.sb([128, G], F32), st.sb([128, G], F32)) for _ in range(2)]
        t1 = [st.sb([128, G], F32) for _ in range(2)]
        t2 = [st.sb([128, G], F32) for _ in range(2)]
        qrot = st.sb([128, 8, G], BF16)
        kk = [[st.sb([128, 4, 128], BF16) for _ in range(2)] for _ in range(2)]
        vk = [st.sb([128, 4, 128], BF16) for _ in range(2)]
        pp = [st.sb([128, 256], BF16) for _ in range(NSB)]
        rrs = [st.sb([64, G], F32) for _ in range(2)]
        oh = [st.sb([64, G], BF16) for _ in range(2)]
        itc = [0]

        def load_g(g):
            gs = slice(g * G, (g + 1) * G)
            S.dma("sp", xg[g % 2][:], fm(xb_d)[:, :, gs], W=[xg[g % 2]])
            S.dma("sp", csg[g % 2][0][:], cos_d[:, gs], W=[csg[g % 2][0]])
            S.dma("sp", csg[g % 2][1][:], sin_d[:, gs], W=[csg[g % 2][1]])

        load_g(0)
        for g in range(NG):
            gs = slice(g * G, (g + 1) * G)
            if g + 1 < NG:
                load_g(g + 1)
            x = xg[g % 2]
            cs, sn = csg[g % 2]
            hf = g % 2
            for m in range(10):
                p1, p2 = nb(), nb()
                if m < 8:
                    gemm_fm(p1, wt, cQ + m * 128, 128, x, G)
                    gemm_fm(p2, wt, cQs + m * 128, 128, x, G)
                else:
                    gemm_fm(p1, wt, cK + (m - 8) * 128, 128, x, G)
                    gemm_fm(p2, wt, cKs + (m - 8) * 128, 128, x, G)
                a, b_ = t1[m % 2], t2[m % 2]
                S.dve(lambda e, a=a, p1=p1: e.tensor_tensor(out=a[:], in0=p1[:], in1=cs[:], op=ALU.mult), R=[p1, cs], W=[a])
                S.dve(lambda e, b_=b_, p2=p2: e.tensor_tensor(out=b_[:], in0=p2[:], in1=sn[:], op=ALU.mult), R=[p2, sn], W=[b_])
                if m < 8:
                    S.pool(lambda e, a=a, b_=b_, m=m: e.tensor_tensor(out=qrot[:, m, :], in0=a[:], in1=b_[:], op=ALU.add),
                           R=[a, b_], W=[qrot])
                else:
                    kt_ = kk[m - 8][hf]
                    S.pool(lambda e, a=a, b_=b_, kt_=kt_: e.tensor_tensor(out=kt_[:].rearrange("p a b -> p (a b)"), in0=a[:], in1=b_[:],
                                                                         op=ALU.add), R=[a, b_], W=[kt_])
            for tt in range(4):
                pv = nb()
                for kc in range(KC):
                    S.pe(lambda e, kc=kc, pv=pv, tt=tt: e.matmul(pv[:, 0:128], lhsT=x[:, kc, tt * 128:(tt + 1) * 128], rhs=wt[:, kc, cV:cV + 128],
                                                                 start=(kc == 0), stop=(kc == KC - 1)), R=[x, wt], W=[pv])
                S.act(lambda e, pv=pv, tt=tt: e.activation(out=vk[hf][:, tt, :], in_=pv[:, 0:128], func=AF.Identity), R=[pv], W=[vk[hf]])
            items = [(hq, s_) for hq in range(16) for s_ in range(5) if not (s_ == 0 and g == 0)]

            def qk(hq, s_):
                m, po_ = hq // 2, (hq % 2) * 64
                kv = hq // 8
                if s_ == 0:
                    ktile, vtile, sl = kk[kv][1 - hf], vk[1 - hf], 3
                else:
                    ktile, vtile, sl = kk[kv][hf], vk[hf], s_ - 1
                c0 = max(0, (s_ - 1) * 128)
                c1 = min(G, (s_ + 1) * 128)
                n = c1 - c0
                psx = pSb[itc[0] % NSB]
                px = pp[itc[0] % NSB]
                itc[0] += 1
                S.pe(lambda e: e.matmul(psx[:, 0:n], lhsT=ktile[po_:po_ + 64, sl, :], rhs=qrot[po_:po_ + 64, m, c0:c1],
                                        start=True, stop=True), R=[ktile, qrot], W=[psx])
                S.act(lambda e: e.activation(out=px[:, 0:n], in_=psx[:, 0:n], func=AF.Exp, scale=0.125), R=[psx], W=[px])
                if s_ >= 1:
                    S.dve(lambda e: e.memset(px[64:128, 0:64], 0.0), R=[px], W=[px])
                if s_ <= 3:
                    off = 128 if s_ >= 1 else 0
                    S.dve(lambda e: e.memset(px[0:64, off + 64:off + 128], 0.0), R=[px], W=[px])
                return (px, vtile, sl, c0, c1, n, kv)

            def pv(hq, s_, st_):
                px, vtile, sl, c0, c1, n, kv = st_
                pO, pL = pOL[hq % 2]
                first = (s_ == 0) or (s_ == 1 and g == 0)
                S.pe(lambda e: e.matmul(pO[0:64, c0:c1], lhsT=vtile[:, sl, kv * 64:(kv + 1) * 64], rhs=px[:, 0:n],
                                        start=first, stop=(s_ == 4)), R=[vtile, px], W=[pO])
                S.pe(lambda e: e.matmul(pL[0:64, c0:c1], lhsT=ones_b[:, 0:64], rhs=px[:, 0:n],
                                        start=first, stop=(s_ == 4)), R=[ones_b, px], W=[pL])
                if s_ == 4:
                    rr = rrs[hq % 2]
                    S.act(lambda e: e.activation(out=rr[:], in_=pL[0:64, :], func=AF.Identity, bias=sk[0:64, hq:hq + 1]),
                          R=[pL, sk], W=[rr])
                    S.dve(lambda e: e.reciprocal(out=rr[:], in_=rr[:]), R=[rr], W=[rr])
                    o = oh[hq % 2]
                    S.dve(lambda e: e.tensor_tensor(out=o[:], in0=pO[0:64, :], in1=rr[:], op=ALU.mult), R=[pO, rr], W=[o])
                    S.dma("sp", mix_d[hq * 64:(hq + 1) * 64, gs], o[:], R=[o])

            LA = NSB - 1
            pend = []
            nxt_i = 0
            for ii, (hq, s_) in enumerate(items):
                while nxt_i < len(items) and nxt_i <= ii + LA - 1:
                    pend.append(qk(*items[nxt_i]))
                    nxt_i += 1
                pv(hq, s_, pend.pop(0))
        st.close()

    def stage_a(i):
        j = i // 2
        st = Stage()
        w_o = (even_w_out if i % 2 == 0 else odd_w_out)[j]
        wo = st.sb([128, KC, D], BF16)
        wq = st.sb([128, KC, D], BF16)
        wm = st.sb([128, KC, D], BF16)
        mk = st.sb([128, KC, MEM], BF16)
        mv = st.sb([128, 2, D], BF16)
        banks = [st.ps() for _ in range(8)]
        wkv = st.sb([128, KC, 2 * D], BF16)
        memb = st.sb([128, KC, MEM], BF16)
        load_w(wkv, 0, mem_w_kv[i], D, 0, 2 * D)
        for kc in range(KC):
            S.dma("pool", memb[:, kc, :], memT_in[kc * 128:(kc + 1) * 128, :], W=[memb])
        for m in range(8):
            p = banks[m % 4]
            gemm_fm(p, wkv, m * 128, 128, memb, MEM)
            S.act(lambda e, p=p, m=m: e.activation(out=mk[:, m, :], in_=p[:, 0:MEM], func=AF.Identity), R=[p], W=[mk])
        for mt in range(2):
            for n in range(2):
                p = banks[4 + (mt * 2 + n) % 4]
                for kc in range(KC):
                    S.pe(lambda e, p=p, kc=kc, mt=mt, n=n: e.matmul(p[:, :], lhsT=memb[:, kc, mt * 128:(mt + 1) * 128],
                                                                  rhs=wkv[:, kc, D + n * 512:D + (n + 1) * 512],
                                                                  start=(kc == 0), stop=(kc == KC - 1)), R=[memb, wkv], W=[p])
                S.act(lambda e, p=p, mt=mt, n=n: e.activation(out=mv[:, mt, n * 512:(n + 1) * 512], in_=p[:], func=AF.Identity),
                      R=[p], W=[mv])
        load_w(wo, 0, w_o, D, 0, D)
        load_w(wq, 0, mem_w_q[i], D, 0, D)
        load_w(wm, 0, mem_w_out[i], D, 0, D)
        mixg = [st.sb([128, KC, G], BF16) for _ in range(2)]
        xf = [st.sb([128, KC, G], F32) for _ in range(2)]
        xbt = st.sb([128, KC, G], BF16)
        qm = st.sb([128, KC, G], BF16)
        om = st.sb([128, KC, G], BF16)
        pmts = [[st.sb([128, G], BF16) for _ in range(2)] for _ in range(2)]
        itc = [0]
        rl = st.sb([128, G], F32)
        tmp = tuple(st.sb([128, G], F32) for _ in range(5))
        bk = [0]

        def nb():
            b = (banks[7], banks[3], banks[2])[bk[0] % 3]
            bk[0] += 1
            return b
        pS0, pS1, pO0, pO1, pLb = banks[0], banks[1], banks[4], banks[5], banks[6]

        def load_g(g):
            gs = slice(g * G, (g + 1) * G)
            S.dma("sp", mixg[g % 2][:], fm(mix_d)[:, :, gs], W=[mixg[g % 2]])
            S.dma("sp", xf[g % 2][:], fm(xres)[:, :, gs], W=[xf[g % 2]])

        load_g(0)
        for g in range(NG):
            gs = slice(g * G, (g + 1) * G)
            if g + 1 < NG:
                load_g(g + 1)
            mx, x = mixg[g % 2], xf[g % 2]
            if dbg:
                dt_ = tmp[0]
                for c in range(KC):
                    S.dve(lambda e, c=c: e.tensor_copy(out=dt_[:], in_=mx[:, c, :]), R=[mx], W=[dt_])
                    S.dma("sp", dbg_aps[(i, "mix")][c * 128:(c + 1) * 128, gs], dt_[:], R=[dt_])
            for m in range(KC):
                p = nb()
                gemm_fm(p, wo, m * 128, 128, mx, G)
                xm = x.sub(m)
                S.dve(lambda e: e.scalar_tensor_tensor(out=xm[:], in0=xm[:], scalar=ALPHA, in1=p[:],
                                                       op0=ALU.mult, op1=ALU.add), R=[xm, p], W=[xm])
            layer_norm(st, x, i, 0, pS0, pS1, x, xbt, tmp, G)
            for m in range(KC):
                p = nb()
                gemm_fm(p, wq, m * 128, 128, xbt, G)
                S.act(lambda e, p=p, m=m: e.activation(out=qm[:, m, :], in_=p[:], func=AF.Identity), R=[p], W=[qm])
            def sc(hd):
                pair = ((banks[0], banks[1]), (banks[2], banks[3]))[itc[0] % 2]
                pm = pmts[itc[0] % 2]
                itc[0] += 1
                for mt in range(2):
                    psx = pair[mt]
                    for jj in range(2):
                        S.pe(lambda e: e.matmul(psx[:], lhsT=mk[:, 2 * hd + jj, mt * 128:(mt + 1) * 128],
                                                rhs=qm[:, 2 * hd + jj, :], start=(jj == 0), stop=(jj == 1)), R=[mk, qm], W=[psx])
                    pmx = pm[mt]
                    S.act(lambda e: e.activation(out=pmx[:], in_=psx[:], func=AF.Exp, scale=1.0 / 16), R=[psx], W=[pmx])
                return pm

            def pvl(hd, pm):
                for mt in range(2):
                    pmx = pm[mt]
                    S.pe(lambda e: e.matmul(pLb[:], lhsT=ones_b[:], rhs=pmx[:], start=(mt == 0), stop=(mt == 1)),
                         R=[ones_b, pmx], W=[pLb])
                    for dc, pOx in ((0, pO0), (1, pO1)):
                        S.pe(lambda e: e.matmul(pOx[:], lhsT=mv[:, mt, hd * 256 + dc * 128:hd * 256 + (dc + 1) * 128],
                                                rhs=pmx[:], start=(mt == 0), stop=(mt == 1)), R=[mv, pmx], W=[pOx])
                S.dve(lambda e: e.reciprocal(out=rl[:], in_=pLb[:]), R=[pLb], W=[rl])
                for dc, pOx in ((0, pO0), (1, pO1)):
                    oc = om.sub(2 * hd + dc)
                    S.dve(lambda e: e.tensor_tensor(out=oc[:], in0=pOx[:], in1=rl[:], op=ALU.mult), R=[pOx, rl], W=[oc])

            cur = sc(0)
            for hd in range(4):
                nxt = sc(hd + 1) if hd + 1 < 4 else None
                pvl(hd, cur)
                cur = nxt
            for m in range(KC):
                p = nb()
                gemm_fm(p, wm, m * 128, 128, om, G)
                xm = x.sub(m)
                S.dve(lambda e: e.scalar_tensor_tensor(out=xm[:], in0=xm[:], scalar=ALPHA, in1=p[:],
                                                       op0=ALU.mult, op1=ALU.add), R=[xm, p], W=[xm])
            layer_norm(st, x, i, 1, pS0, pS1, x, xbt, tmp, G)
            S.dma("sp", fm(xres2)[:, :, gs], x[:], R=[x])
            S.dma("sp", fm(xb2_d)[:, :, gs], xbt[:], R=[xbt])
            if dbg:
                S.dma("sp", fm(dbg_aps[(i, "x2")])[:, :, gs], x[:], R=[x])
        st.close()

    def stage_b(i, last):
        st = Stage()
        win = st.sb([128, KC, 2 * DFF], BF16)
        wout = st.sb([128, NJ, D], BF16)
        wblk = {}
        for j0 in range(0, NJ, 2):
            for half in range(2):
                c0_ = half * DFF + j0 * 128
                v_ = Tl(win.ap[:, :, c0_:c0_ + 256])
                load_w(v_, 0, ffn_w_in[i], D, c0_, 256)
                wblk[(j0, half)] = v_
        load_w(wout, 0, ffn_w_out[i], DFF, 0, D)
        banks = [st.ps() for _ in range(8)]
        xb_t = [st.sb([128, KC, 2 + GB], BF16) for _ in range(2)]
        xf = [st.sb([128, KC, GB], F32) for _ in range(2)]
        xo_b = st.sb([128, KC, GB], BF16)
        hb = st.sb([128, NJ, GB], BF16)
        cv = [st.sb([128, GB], F32) for _ in range(4)]
        ge = [st.sb([128, GB], F32) for _ in range(4)]
        tmp = tuple(st.sb([128, GB], F32) for _ in range(5))
        bk = [0]

        def nb():
            b = banks[bk[0] % 8]
            bk[0] += 1
            return b

        def load_g(g):
            t = xb_t[g % 2]
            if g == 0:
                S.dve(lambda e: e.memset(t[:, :, 0:2], 0.0), W=[t])
                S.dma("sp", t[:, :, 2:2 + GB], fm(xb2_d)[:, :, 0:GB], W=[t])
            else:
                S.dma("sp", t[:], fm(xb2_d)[:, :, g * GB - 2:(g + 1) * GB], W=[t])
            S.dma("sp", xf[g % 2][:], fm(xres2)[:, :, g * GB:(g + 1) * GB], W=[xf[g % 2]])

        def cw(k, jj):
            return colap("conv_w", (i * 3 + k) * NJ + jj)

        load_g(0)
        for g in range(NGB):
            gs = slice(g * GB, (g + 1) * GB)
            if g + 1 < NGB:
                load_g(g + 1)
            xb_, x = xb_t[g % 2], xf[g % 2]
            for j0 in range(0, NJ, 2):
                pr_ = []
                for jj in (j0, j0 + 1):
                    pg, pu = nb(), nb()
                    gemm_fm(pg, wblk[(j0, 0)], (jj - j0) * 128, 128, xb_, GB + 2, xs=slice(0, GB + 2))
                    gemm_fm(pu, wblk[(j0, 1)], (jj - j0) * 128, 128, xb_, GB, xs=slice(2, GB + 2))
                    pr_.append((jj, pg, pu, cv[jj % 4], ge[jj % 4]))
                for (jj, pg, pu, c_, g_) in pr_:
                    S.act(lambda e: e.activation(out=c_[:], in_=pg[:, 2:2 + GB], func=AF.Identity,
                                                 scale=cw(2, jj), bias=colap("conv_b", i * NJ + jj)), R=[pg, cols], W=[c_])
                for (jj, pg, pu, c_, g_) in pr_:
                    S.dve(lambda e: e.scalar_tensor_tensor(out=c_[:], in0=pg[:, 1:1 + GB], scalar=cw(1, jj), in1=c_[:],
                                                           op0=ALU.mult, op1=ALU.add), R=[pg, c_, cols], W=[c_])
                for (jj, pg, pu, c_, g_) in pr_:
                    S.dve(lambda e: e.scalar_tensor_tensor(out=c_[:], in0=pg[:, 0:GB], scalar=cw(0, jj), in1=c_[:],
                                                           op0=ALU.mult, op1=ALU.add), R=[pg, c_, cols], W=[c_])
                for (jj, pg, pu, c_, g_) in pr_:
                    S.act(lambda e: e.activation(out=g_[:], in_=c_[:], func=AF.Gelu_apprx_tanh), R=[c_], W=[g_])
                for (jj, pg, pu, c_, g_) in pr_:
                    hj = hb.sub(jj)
                    S.dve(lambda e: e.tensor_tensor(out=hj[:], in0=pu[:, 0:GB], in1=g_[:], op=ALU.mult), R=[pu, g_], W=[hj])
            for m in range(KC):
                p = nb()
                for jj in range(NJ):
                    S.pe(lambda e: e.matmul(p[:, 0:GB], lhsT=wout[:, jj, m * 128:(m + 1) * 128], rhs=hb[:, jj, :],
                                            start=(jj == 0), stop=(jj == NJ - 1)), R=[wout, hb], W=[p])
                xm = x.sub(m)
                S.dve(lambda e: e.scalar_tensor_tensor(out=xm[:], in0=xm[:], scalar=ALPHA, in1=p[:, 0:GB],
                                                       op0=ALU.mult, op1=ALU.add), R=[xm, p], W=[xm])
            pM0, pM1 = nb(), nb()
            layer_norm(st, x, i, 2, pM0, pM1, x, xo_b, tmp, GB)
            if last:
                S.dma("sp", fm(out_ap)[:, :, gs], x[:], R=[x])
            else:
                S.dma("sp", fm(xres)[:, :, gs], x[:], R=[x])
                S.dma("sp", fm(xb_d)[:, :, gs], xo_b[:], R=[xo_b])
            if dbg:
                S.dma("sp", fm(dbg_aps[(i, "x3")])[:, :, gs], x[:], R=[x])
        st.close()

    for i in range(nlayers):
        if i % 2 == 0:
            stage_c_even(i)
            stage_m_diff(i)
        else:
            stage_c_odd(i)
        stage_a(i)
        stage_b(i, last=(i == nlayers - 1))
    cst.es.close()
    es0.close()
    return nc


W_NAMES = ["even_w_in", "even_w_out", "gla_gate_w", "diff_lam_q1", "diff_lam_k1", "diff_lam_q2", "diff_lam_k2",
           "odd_w_in", "odd_w_out", "swa_sinks", "mem_w_q", "mem_w_kv", "mem_w_out", "ffn_w_in", "ffn_w_out"]


def make_in_map(inp, b, cols):
    m = {
        "xT": np.ascontiguousarray(np.asarray(inp["x"][b], np.float32).T),
        "memT": np.ascontiguousarray(np.asarray(inp["mem"][b], np.float32).T),
        "pos": np.ascontiguousarray(np.asarray(inp["positions"][b], np.int32).reshape(1, -1)),
        "cols": cols,
    }
    for n in W_NAMES:
        m[n] = np.ascontiguousarray(np.asarray(inp[n], np.float32))
    return m


def kernel(**inputs):
    x = np.asarray(inputs["x"])
    B, T, _ = x.shape
    cols = pack_cols(inputs)
    nc = build(T)
    in_maps = [make_in_map(inputs, b % B, cols) for b in range(8)]
    res = run_bass_kernel_spmd(nc, in_maps, core_ids=list(range(8)))
    out = np.stack([np.asarray(res.results[b]["outT"], np.float32).T for b in range(B)], axis=0)
    return out.astype(np.float32)
```
